# Optimizing a Trainium2 kernel written in Bass

```python
import jax, jax.numpy as jnp
from jax import lax
import numpy as np

D_MODEL = 2048
BATCH = 4
SEQ = 4096
DEPTH = 1

CHUNK = 64
D_MIX = D_MODEL
GMLP_WIDTH = D_MIX // 2
GMLP_HEADS = 8
GMLP_HEAD_DIM = GMLP_WIDTH // GMLP_HEADS
GMLP_BLOCK = 128
CONV_CH = D_MIX - GMLP_WIDTH
CONV_GROUPS = 8
CONV_K = 3
D_IN_PROJ = 2 * GMLP_WIDTH + 3 * CONV_CH
N_GROUPS = 4
EXPERTS_PER_GROUP = 8
N_EXPERTS = N_GROUPS * EXPERTS_PER_GROUP
TOP_K_IN_GROUP = 2
D_EXPERT = 512
MOE_BLOCK = 128
EPS = 1e-6

kernel_name = "hymba_gmlp_shortconv_hiermoe_block"


def rmsnorm(x, g):
    xf = x.astype(jnp.float32)
    y = xf * lax.rsqrt(jnp.mean(xf * xf, axis=-1, keepdims=True) + EPS)
    return (y * g.astype(jnp.float32)).astype(x.dtype)


def gmlp_mixer(u, v, v_norm_g, ws, bs):
    b_, s_, _ = v.shape
    nb = s_ // GMLP_BLOCK
    v = rmsnorm(v.reshape(b_, s_, GMLP_HEADS, GMLP_HEAD_DIM), v_norm_g.reshape(GMLP_HEADS, GMLP_HEAD_DIM))
    v = v.reshape(b_, nb, GMLP_BLOCK, GMLP_HEADS, GMLP_HEAD_DIM)
    pos = jnp.arange(GMLP_BLOCK)
    mask = (pos[:, None] // CHUNK) >= (pos[None, :] // CHUNK)
    w = jnp.where(mask[None], ws, 0).astype(v.dtype)
    mixed = jnp.einsum('hij,bnjhd->bnihd', w, v) + bs.T.astype(v.dtype)[None, None, :, :, None]
    return u * mixed.reshape(b_, s_, GMLP_WIDTH)


def conv_mixer(b_gate, c_gate, hv, conv_w):
    z = c_gate * hv
    s_ = z.shape[1]
    zp = jnp.pad(z, ((0, 0), (CONV_K - 1, 0), (0, 0)))
    conv = sum(conv_w[k] * zp[:, k:k + s_, :] for k in range(CONV_K))
    return b_gate * conv


def hier_moe(x, wgr, bgr, wer, ber, w_gate, w_up, w_down):
    t_ = x.shape[0]
    d_ = x.shape[1]
    p_g = jax.nn.softmax((x @ wgr + bgr).astype(jnp.float32), axis=-1)
    pg_top, g_idx = lax.top_k(p_g, 1)
    logits_e = (x @ wer + ber).astype(jnp.float32).reshape(t_, N_GROUPS, EXPERTS_PER_GROUP)
    le = jnp.take_along_axis(logits_e, g_idx[:, :, None], axis=1)[:, 0]
    q = jax.nn.softmax(le, axis=-1)
    q_top, e_local = lax.top_k(q, TOP_K_IN_GROUP)
    q_top = q_top / jnp.sum(q_top, axis=-1, keepdims=True)
    gate = pg_top * q_top
    expert = g_idx * EXPERTS_PER_GROUP + e_local

    n_assign = t_ * TOP_K_IN_GROUP
    e_flat = expert.reshape(n_assign)
    tok_flat = jnp.repeat(jnp.arange(t_, dtype=jnp.int32), TOP_K_IN_GROUP)
    w_flat = gate.reshape(n_assign)
    order = jnp.argsort(e_flat)
    e_s = e_flat[order]
    tok_s = tok_flat[order]
    w_s = w_flat[order]
    counts = jnp.bincount(e_flat, length=N_EXPERTS)
    start = jnp.cumsum(counts) - counts
    padded = (counts + MOE_BLOCK - 1) // MOE_BLOCK * MOE_BLOCK
    pad_end = jnp.cumsum(padded)
    pad_start = pad_end - padded
    dest = pad_start[e_s] + jnp.arange(n_assign) - start[e_s]
    n_blocks = -(-n_assign // MOE_BLOCK) + N_EXPERTS
    rows = n_blocks * MOE_BLOCK
    buf_tok = jnp.zeros((rows,), jnp.int32).at[dest].set(tok_s)
    buf_w = jnp.zeros((rows,), jnp.float32).at[dest].set(w_s)
    blk_expert = jnp.minimum(
        jnp.searchsorted(pad_end, jnp.arange(n_blocks) * MOE_BLOCK, side='right'), N_EXPERTS - 1)

    def run_block(args):
        toks, e = args
        xb = x[toks]
        hb = jax.nn.silu(xb @ w_gate[e]) * (xb @ w_up[e])
        return hb @ w_down[e]

    out = lax.map(run_block, (buf_tok.reshape(n_blocks, MOE_BLOCK), blk_expert))
    out = out.reshape(rows, d_) * buf_w[:, None].astype(x.dtype)
    return jnp.zeros_like(x).at[buf_tok].add(out)


def setup_inputs(seed: int = 0) -> dict:
    key = jax.random.key(seed)
    ks = jax.random.split(key, 24)
    L = DEPTH

    def nrm(k, shape, scale):
        return jax.random.normal(k, shape, jnp.float32) * scale

    def gain(k, shape):
        return 1.0 + nrm(k, shape, 0.02)

    return {
        'x': nrm(ks[0], (BATCH, SEQ, D_MODEL), 1.0),
        'norm_mix_g': gain(ks[1], (L, D_MODEL)),
        'w_in': nrm(ks[2], (L, D_MODEL, D_IN_PROJ), D_MODEL ** -0.5),
        'gmlp_v_norm_g': gain(ks[3], (L, GMLP_WIDTH)),
        'gmlp_ws': nrm(ks[4], (L, GMLP_HEADS, GMLP_BLOCK, GMLP_BLOCK), GMLP_BLOCK ** -0.5),
        'gmlp_bs': gain(ks[5], (L, GMLP_HEADS, GMLP_BLOCK)),
        'conv_w': nrm(ks[6], (L, CONV_K, CONV_CH), CONV_K ** -0.5),
        'out_norm_gmlp_g': gain(ks[7], (L, GMLP_WIDTH)),
        'out_norm_conv_g': gain(ks[8], (L, CONV_CH)),
        'w_out': nrm(ks[9], (L, D_MIX, D_MODEL), D_MIX ** -0.5),
        'norm_ffn_g': gain(ks[10], (L, D_MODEL)),
        'router_group_w': nrm(ks[11], (L, D_MODEL, N_GROUPS), D_MODEL ** -0.5),
        'router_group_b': nrm(ks[12], (L, N_GROUPS), 0.01),
        'router_expert_w': nrm(ks[13], (L, D_MODEL, N_EXPERTS), D_MODEL ** -0.5),
        'router_expert_b': nrm(ks[14], (L, N_EXPERTS), 0.01),
        'expert_w_gate': nrm(ks[15], (L, N_EXPERTS, D_MODEL, D_EXPERT), D_MODEL ** -0.5),
        'expert_w_up': nrm(ks[16], (L, N_EXPERTS, D_MODEL, D_EXPERT), D_MODEL ** -0.5),
        'expert_w_down': nrm(ks[17], (L, N_EXPERTS, D_EXPERT, D_MODEL), D_EXPERT ** -0.5),
        'norm_final_g': gain(ks[18], (D_MODEL,)),
    }


def reference(x, norm_mix_g, w_in, gmlp_v_norm_g, gmlp_ws, gmlp_bs, conv_w, out_norm_gmlp_g,
              out_norm_conv_g, w_out, norm_ffn_g, router_group_w, router_group_b, router_expert_w,
              router_expert_b, expert_w_gate, expert_w_up, expert_w_down, norm_final_g):
    b_, s_, d_ = x.shape
    splits = [GMLP_WIDTH, 2 * GMLP_WIDTH, 2 * GMLP_WIDTH + CONV_CH, 2 * GMLP_WIDTH + 2 * CONV_CH]
    h = x
    for l in range(DEPTH):
        xn = rmsnorm(h, norm_mix_g[l])
        proj = xn @ w_in[l]
        u, v, bg, cg, hv = jnp.split(proj, splits, axis=-1)
        ya = gmlp_mixer(jax.nn.gelu(u), jax.nn.gelu(v), gmlp_v_norm_g[l], gmlp_ws[l], gmlp_bs[l])
        yb = conv_mixer(bg, cg, hv, conv_w[l])
        y = jnp.concatenate([rmsnorm(ya, out_norm_gmlp_g[l]), rmsnorm(yb, out_norm_conv_g[l])], axis=-1)
        h = h + y @ w_out[l]
        hn = rmsnorm(h, norm_ffn_g[l]).reshape(b_ * s_, d_)
        m = hier_moe(hn, router_group_w[l], router_group_b[l], router_expert_w[l], router_expert_b[l],
                     expert_w_gate[l], expert_w_up[l], expert_w_down[l])
        h = h + m.reshape(b_, s_, d_)
    return rmsnorm(h, norm_final_g)
```

```python
import numpy as np
import concourse.bass as bass
import concourse.mybir as mybir
from contextlib import ExitStack

F32 = mybir.dt.float32
BF16 = mybir.dt.bfloat16
I32 = mybir.dt.int32
U32 = mybir.dt.uint32
AF = mybir.ActivationFunctionType
ALU = mybir.AluOpType
AX = mybir.AxisListType

ENGS = ("pe", "act", "dve", "pool", "sp")
NDMASEM = {"sp": 12, "act": 8, "pool": 20}


class Buf:
    __slots__ = ("name", "w", "r")

    def __init__(self, name=""):
        self.name = name
        self.w = None
        self.r = []


class Sched:
    def __init__(self, nc, es):
        self.nc = nc
        self.es = es
        self.ops = {e: [] for e in ENGS}
        self.ndma = {q: 0 for q in NDMASEM}
        self.nsb = 0

    def buf(self, name=""):
        return Buf(name)

    def bufs(self, n, name=""):
        return [Buf(f"{name}{i}") for i in range(n)]

    def sb(self, shape, dtype, name=None):
        self.nsb += 1
        return self.es.enter_context(self.nc.sbuf_tensor(name or f"sb{self.nsb}", list(shape), dtype))

    def ps(self, shape, dtype, name=None):
        self.nsb += 1
        return self.es.enter_context(self.nc.psum_tensor(name or f"ps{self.nsb}", list(shape), dtype))

    def _deps(self, reads, writes):
        deps = []
        for b in reads:
            if b.w is not None:
                deps.append(b.w)
        for b in writes:
            if b.w is not None:
                deps.append(b.w)
            deps.extend(b.r)
        return deps

    def _commit(self, ev, reads, writes):
        for b in reads:
            b.r.append(ev)
        for b in writes:
            b.w = ev
            b.r = []

    def op(self, eng, fn, reads=(), writes=(), pe_acc=False):
        deps = self._deps(reads, writes)
        if pe_acc:
            deps = [d for d in deps if not (d[0] == "c" and d[1] == "pe")]
        idx = len(self.ops[eng])
        ev = ("c", eng, idx)
        self.ops[eng].append(("c", fn, deps, None))
        self._commit(ev, reads, writes)
        return ev

    def dma(self, q, fn, reads=(), writes=()):
        deps = self._deps(reads, writes)
        j = self.ndma[q]
        self.ndma[q] += 1
        K = NDMASEM[q]
        if j >= K:
            deps.append(("d", q, j - K))
        idx = len(self.ops[q])
        ev = ("d", q, j)
        self.ops[q].append(("d", fn, deps, j))
        self._commit(ev, reads, writes)
        return ev

    def emit(self, final_events=()):
        nc = self.nc
        es = self.es
        csem = {e: es.enter_context(nc.semaphore(f"c_{e}")) for e in ("pe", "act", "dve", "pool")}
        dsem = {q: [es.enter_context(nc.semaphore(f"d_{q}{i}")) for i in range(K)] for q, K in NDMASEM.items()}
        waits = {e: [] for e in ENGS}
        signal = {e: set() for e in ("pe", "act", "dve", "pool")}
        for e in ENGS:
            seen_c = {}
            seen_d = {}
            for (kind, fn, deps, j) in self.ops[e]:
                wc = {}
                wd = {}
                for d in deps:
                    if d[0] == "c":
                        if d[1] == "pe" and e == "pe":
                            continue
                        wc[d[1]] = max(wc.get(d[1], -1), d[2])
                    else:
                        q, jj = d[1], d[2]
                        K = NDMASEM[q]
                        key = (q, jj % K)
                        wd[key] = max(wd.get(key, -1), jj // K)
                w = []
                for x, i in wc.items():
                    if seen_c.get(x, -1) >= i:
                        continue
                    seen_c[x] = i
                    signal[x].add(i)
                    w.append(("c", x, i))
                for key, u in wd.items():
                    if seen_d.get(key, -1) >= u:
                        continue
                    seen_d[key] = u
                    w.append(("d", key[0], key[1], u))
                waits[e].append(w)
        cnt = {}
        for x in signal:
            s = sorted(signal[x])
            cnt[x] = {i: k + 1 for k, i in enumerate(s)}
        engobj = {"pe": nc.tensor, "act": nc.scalar, "dve": nc.vector, "pool": nc.gpsimd, "sp": nc.sync}
        self.stats = {e: len(self.ops[e]) for e in ENGS}

        def run2(e, eng):
            for idx, ((kind, fn, deps, j), w) in enumerate(zip(self.ops[e], waits[e])):
                for ww in w:
                    if ww[0] == "c":
                        eng.wait_ge(csem[ww[1]], cnt[ww[1]][ww[2]])
                    else:
                        eng.wait_ge(dsem[ww[1]][ww[2]], 16 * (ww[3] + 1))
                inst = fn(eng)
                if kind == "d":
                    K = NDMASEM[e]
                    inst.then_inc(dsem[e][j % K], 16)
                elif inst is not None and idx in signal.get(e, ()):
                    inst.then_inc(csem[e], 1)

        with nc.Block() as block:
            @block.tensor
            def _(eng):
                run2("pe", eng)

            @block.scalar
            def _(eng):
                run2("act", eng)

            @block.vector
            def _(eng):
                run2("dve", eng)

            @block.gpsimd
            def _(eng):
                run2("pool", eng)

            @block.sync
            def _(eng):
                run2("sp", eng)


def _barrier(self):
    evs = []
    for e in ("pe", "act", "dve", "pool"):
        ops = self.ops[e]
        for i in range(len(ops) - 1, -1, -1):
            if ops[i][0] == "c" and ops[i][3] != "nop":
                evs.append(("c", e, i))
                break
    for q, K in NDMASEM.items():
        n = self.ndma[q]
        for j in range(max(0, n - K), n):
            evs.append(("d", q, j))
    for e in ENGS:
        self.ops[e].append(("c", (lambda eng: None), list(evs), "nop"))


Sched.barrier = _barrier

_DSZ = {F32: 4, BF16: 2, I32: 4, U32: 4}


class Arena:
    def __init__(self, S, nwords, name="arena"):
        self.t = S.sb([128, nwords], F32, name)
        self.n = nwords
        self.off = 0

    def alloc(self, free_elems, dtype):
        nw = (free_elems * _DSZ[dtype] + 3) // 4
        nw = (nw + 7) // 8 * 8
        assert self.off + nw <= self.n, ("arena overflow", self.off, nw, self.n)
        v = self.t[:, self.off:self.off + nw]
        self.off += nw
        if dtype != F32:
            v = v.bitcast(dtype)
        if v.shape[1] != free_elems:
            v = v[:, 0:free_elems]
        return v

    def alloc3(self, a, b, dtype):
        return self.alloc(a * b, dtype).rearrange("p (a b) -> p a b", a=a)


class Rot:
    def __init__(self, S, arena, n, free_elems, dtype, a=None):
        if a is None:
            self.t = [arena.alloc(free_elems, dtype) for _ in range(n)]
        else:
            self.t = [arena.alloc3(a, free_elems // a, dtype) for _ in range(n)]
        self.b = S.bufs(n)
        self.n = n
        self.i = -1

    def next(self):
        self.i += 1
        j = self.i % self.n
        return self.t[j], self.b[j]

from concourse.bass_utils import run_bass_kernel_spmd

D = 2048
NT = 2048
TT = 16
CAP = 384
NE = 32
EPS = 1e-6
BIG = 1.0e4
PHASES = 99
N_PRE = 4


def build_nc(phases=PHASES, debug=False):
    nc = bass.Bass("TRN2", target_bir_lowering=False)

    def din(name, shape, dt=F32):
        return nc.dram_tensor(name, list(shape), dt, kind="ExternalInput").ap()

    x_d = din("x", [NT, D])
    xh_d = din("xh", [2, D])
    gmix_d = din("gmix", [128, D])
    gffn_d = din("gffn", [128, D])
    gfin_d = din("gfin", [128, D])
    gvn_d = din("gvn", [128, 1024])
    ga_d = din("ga", [128, 1024])
    gb_d = din("gb", [128, 8])
    wsT_d = din("wsT", [128, 8, 128])
    bsT_d = din("bsT", [128, 8])
    cw_d = din("cw", [128, 8, 3])
    win_d = din("w_in", [D, 5120])
    wout_d = din("w_out", [D, D])
    wr_d = din("wr", [128, 16, 36])
    rb_d = din("rb", [128, 36])
    if phases >= 4:
        wg_d = din("wg", [NE, D, 512])
        wu_d = din("wu", [NE, D, 512])
        wd_d = din("wd", [NE, 512, D])
    IK = "ExternalOutput" if debug else "Internal"
    if debug:
        dbg_d = nc.dram_tensor("dbg", [128, 16 * NT], BF16, kind=("ExternalOutput" if phases == 0 else "Internal")).ap()
        dbg2_d = nc.dram_tensor("dbg2", [128, 160], F32, kind="ExternalOutput").ap()
    out_d = nc.dram_tensor("out", [NT, D], F32, kind="ExternalOutput").ap()
    yT_d = nc.dram_tensor("yT_s", [TT, 128, 16, 128], BF16, kind=(IK if phases in (1, 2) else "Internal")).ap()
    h_d = nc.dram_tensor("h_s", [NT, D], F32, kind=(IK if phases == 3 else "Internal")).ap()
    xs_d = nc.dram_tensor("xs_s", [NE * CAP, D], BF16, kind=(IK if phases == 3 else "Internal")).ap()
    Y_d = nc.dram_tensor("Y_s", [NE * CAP, D], BF16, kind=(IK if phases == 4 else "Internal")).ap()

    NPRE = N_PRE if phases >= 4 else 0
    if NPRE:
        wgb_d = nc.dram_tensor("wgb_s", [NPRE, D, 512], BF16, kind="Internal").ap()
        wub_d = nc.dram_tensor("wub_s", [NPRE, D, 512], BF16, kind="Internal").ap()
        wdb_d = nc.dram_tensor("wdb_s", [NPRE, 512, D], BF16, kind="Internal").ap()

    with ExitStack() as es:
        S = Sched(nc, es)
        mult, add = ALU.mult, ALU.add

        P = Arena(S, 8700, "persist")
        ident = P.alloc(128, BF16); identb = S.buf()
        utri = P.alloc(128, BF16); utrib = S.buf()
        ones = P.alloc(128, BF16); onesb = S.buf()
        onesf = P.alloc(8, F32); onesfb = S.buf()
        iotc = P.alloc(32, F32); iotcb = S.buf()
        gslot = P.alloc(D, F32); gslotb = S.buf()
        gvn = P.alloc(1024, F32); gvnb = S.buf()
        ga = P.alloc(1024, F32); gab = S.buf()
        gbt = P.alloc(8, F32); gbtb = S.buf()
        wsT = P.alloc3(8, 128, BF16); wsTb = S.buf()
        bsT = P.alloc(8, F32); bsTb = S.buf()
        cw = P.alloc3(8, 3, F32); cwb = S.buf()
        wr = P.alloc3(16, 36, BF16); wrb = S.buf()
        rb = P.alloc(36, F32); rbb = S.buf()
        ssA = P.alloc(32, F32); ssAb = S.buf()
        rsA = P.alloc(16, F32); rsAb = S.buf()
        rsB = P.alloc(16, F32); rsBb = S.buf()
        gates = P.alloc3(16, 2, F32); gatesb = S.bufs(16)
        idx = P.alloc3(16, 2, I32); idxb = S.bufs(16)
        Aall = P.alloc3(16, 32, BF16); Aallb = S.bufs(16)
        xnTh = P.alloc3(16, 2, BF16); xnThb = S.buf()
        junk = P.alloc(D, BF16); junkb = S.buf()
        rstat = P.alloc3(16, 5, F32); rstatb = S.bufs(16)
        gex = P.alloc(80, F32); gexb = S.buf()
        rowid = P.alloc3(NE, CAP // 128, F32); rowidb = S.buf()
        spos = P.alloc(8, F32); sposb = S.buf()
        vidx = P.alloc3(NE, CAP // 128, I32); vidxb = S.buf()
        st = Rot(S, P, 16, 8, F32)
        sm = Rot(S, P, 26, 40, F32)

        PT = [S.ps([128, 1024], BF16, f"pt{i}") for i in range(2)]
        PTb = S.bufs(2)
        PM = [S.ps([128, 512], F32, f"pm{i}") for i in range(6)]
        PMb = S.bufs(6)

        def dma(q, out, in_, reads, writes):
            S.dma(q, lambda e: e.dma_start(out=out, in_=in_), reads=reads, writes=writes)

        def finish():
            S.barrier()
            S.emit()

        bcreg = [None]

        def _mkreg(e):
            bcreg[0] = e.to_reg(NE * CAP - 1)
            return None
        S.op("pool", _mkreg)
        S.op("pool", lambda e: e.memset(ident, 0.0), writes=[identb])
        S.op("pool", lambda e: e.affine_select(out=ident, in_=ident, pattern=[[-1, 128]],
                                               compare_op=ALU.not_equal, fill=1.0, base=0,
                                               channel_multiplier=1), reads=[identb], writes=[identb])
        S.op("pool", lambda e: e.memset(ones, 1.0), writes=[onesb])
        S.op("pool", lambda e: e.memset(onesf, 1.0), writes=[onesfb])
        S.op("pool", lambda e: e.memset(utri, 1.0), writes=[utrib])
        S.op("pool", lambda e: e.affine_select(out=utri, in_=utri, pattern=[[1, 128]],
                                               compare_op=ALU.is_gt, fill=0.0, base=0,
                                               channel_multiplier=-1), reads=[utrib], writes=[utrib])
        S.op("pool", lambda e: e.iota(iotc, pattern=[[CAP, 32]], base=0, channel_multiplier=0,
                                      allow_small_or_imprecise_dtypes=True), writes=[iotcb])
        dma("sp", gslot, gmix_d, [], [gslotb])
        dma("sp", gvn, gvn_d, [], [gvnb])
        dma("sp", ga, ga_d, [], [gab])
        dma("sp", gbt, gb_d, [], [gbtb])
        dma("sp", bsT, bsT_d, [], [bsTb])
        dma("sp", cw, cw_d, [], [cwb])
        dma("sp", rb, rb_d, [], [rbb])
        dma("pool", wsT, wsT_d, [], [wsTb])
        dma("pool", wr, wr_d, [], [wrb])
        S.op("dve", lambda e: e.memset(wsT[64:128, :, 0:64], 0.0), reads=[wsTb], writes=[wsTb])

        A = Arena(S, 44200, "arena")
        xnT = A.alloc3(16, NT, BF16); xnTb = S.buf()
        wslot = [A.alloc3(16, 512, BF16) for _ in range(4)]; wslotb = S.bufs(4)
        zt = A.alloc3(2, D, BF16); ztb = S.buf()
        S.op("dve", lambda e: e.memset(zt, 0.0), writes=[ztb])
        NZ = NE * CAP // 256
        zfill_i = [0]

        def zfill(n):
            for _ in range(n):
                zi = zfill_i[0]
                zfill_i[0] += 1
                if zi < NZ:
                    dst = xs_d[zi * 256:(zi + 1) * 256, :]
                elif zi < 2 * NZ:
                    dst = Y_d[(zi - NZ) * 256:(zi - NZ + 1) * 256, :]
                else:
                    return
                dma("sp", dst.rearrange("(p a) d -> p a d", a=2), zt, [ztb], [S.buf()])

        prepend = []
        for j in range(NPRE):
            e_ = j + 1
            for (dstm, srcm) in ((wgb_d[j], wg_d[e_]), (wub_d[j], wu_d[e_])):
                dfl = dstm.rearrange("(p k) n -> p (k n)", k=16)
                sfl = srcm.rearrange("(p k) n -> p (k n)", k=16)
                for q in range(4):
                    prepend.append((dfl[:, q * 2048:(q + 1) * 2048], sfl[:, q * 2048:(q + 1) * 2048]))
            for k in range(4):
                prepend.append((wdb_d[j, k * 128:(k + 1) * 128, :], wd_d[e_, k * 128:(k + 1) * 128, :]))
        pre_i = [0]

        pre_tot = [None]

        def preconv_n(n):
            for _ in range(n):
                if prepend:
                    dst, src = prepend.pop(0)
                    dma("pool", dst, src, [], [S.buf()])

        def preconv(item, nitems=64):
            if pre_tot[0] is None:
                pre_tot[0] = len(prepend)
            while prepend and pre_i[0] * nitems < (item + 1) * pre_tot[0]:
                dst, src = prepend.pop(0)
                pre_i[0] += 1
                dma("pool", dst, src, [], [S.buf()])

        markA = A.off
        win_v = win_d.rearrange("(k p) n -> p k n", p=128)

        def rstd(ss_ap, ssb, n, scale, rows=128):
            sd, sdb = sm.next()
            rs, rsb = sm.next()
            S.op("act", lambda e: e.activation(out=sd[:rows, 0:n], in_=ss_ap, func=AF.Sqrt,
                                               scale=scale, bias=epsb[:rows, 0:1]),
                 reads=[ssb, epsbb], writes=[sdb])
            S.op("dve", lambda e: e.reciprocal(out=rs[:rows, 0:n], in_=sd[:rows, 0:n]),
                 reads=[sdb], writes=[rsb])
            return rs, rsb

        epsb = P.alloc(8, F32); epsbb = S.buf()
        S.op("pool", lambda e: e.memset(epsb, EPS), writes=[epsbb])

        nload = [0]
        NWS = len(wslot)

        def load_w(col0):
            s = nload[0] % NWS
            nload[0] += 1
            dma("pool", wslot[s], win_v[:, :, col0:col0 + 512], [], [wslotb[s]])
            return s

        a1w = {0: (load_w(0), load_w(1024))}
        a2w = {}

        xt = Rot(S, A, 3, D, F32)
        xn = Rot(S, A, 3, D, BF16)

        def a0_s1(t):
            rows = 128 if t < 16 else 2
            src = x_d[t * 128:(t + 1) * 128, :] if t < 16 else xh_d
            xtt, xttb = xt.next()
            xnn, xnnb = xn.next()
            ss, ssb = st.next()
            dma("sp", xtt[:rows], src, [], [xttb])
            if NPRE > 4 and t < 12:
                preconv_n(1)
            S.op("act", lambda e: e.activation(
                out=junk[:rows], in_=xtt[:rows], func=AF.Square, accum_out=ss[:rows, 0:1]),
                reads=[xttb], writes=[junkb, ssb])
            rs, rsb = rstd(ss[:rows, 0:1], ssb, 1, 1.0 / D, rows)
            S.op("dve", lambda e: e.scalar_tensor_tensor(
                out=xnn[:rows], in0=xtt[:rows], scalar=rs[:rows, 0:1], in1=gslot[:rows],
                op0=mult, op1=mult), reads=[xttb, rsb, gslotb], writes=[xnnb])
            return (t, rows, xnn, xnnb)

        def a0_s2(ctx):
            t, rows, xnn, xnnb = ctx
            for half in range(2):
                for kk in range(8):
                    k = half * 8 + kk
                    S.op("pe", lambda e, half=half, kk=kk, k=k: e.transpose(
                        out=PT[half][:, kk * 128:kk * 128 + rows],
                        in_=xnn[:rows, k * 128:(k + 1) * 128], identity=ident[:rows, :rows]),
                        reads=[xnnb, identb], writes=[PTb[half]])
                srcp = PT[half][:, :].rearrange("p (k n) -> p k n", k=8)
                if t < 16:
                    dst = xnT[:, half * 8:(half + 1) * 8, t * 128:(t + 1) * 128]
                    wb = xnTb
                else:
                    dst = xnTh[:, half * 8:(half + 1) * 8, :]
                    srcp = srcp[:, :, 0:2]
                    wb = xnThb
                if half == 0:
                    S.op("act", lambda e, dst=dst, srcp=srcp: e.copy(out=dst, in_=srcp),
                         reads=[PTb[half]], writes=[wb])
                else:
                    S.op("dve", lambda e, dst=dst, srcp=srcp: e.tensor_copy(out=dst, in_=srcp),
                         reads=[PTb[half]], writes=[wb])

        def skew(n, s1, s2):
            prev = None
            for i in range(n):
                cur = s1(i)
                if prev is not None:
                    s2(prev)
                prev = cur
            s2(prev)

        skew(17, a0_s1, a0_s2)

        def skew3(n, s1, s2, s3):
            c1 = {}
            c2 = {}
            for i in range(n + 2):
                if i < n:
                    c1[i] = s1(i)
                if 0 <= i - 1 < n:
                    c2[i - 1] = s2(c1.pop(i - 1))
                if 0 <= i - 2 < n:
                    s3(c2.pop(i - 2))

        if phases == 0:
            if debug:
                dma("sp", dbg_d.rearrange("p (k n) -> p k n", k=16), xnT, [xnTb], [S.buf()])
                dma("sp", dbg2_d[:, 0:32].bitcast(BF16).rearrange("p (k n) -> p k n", k=16)[:, :, 0:2], xnTh, [xnThb], [S.buf()])
            finish()
            return nc
        S.barrier()
        A.off = markA
        gu_r = Rot(S, A, 3, 512, F32)
        gv_r = Rot(S, A, 3, 512, F32)
        sq_r = Rot(S, A, 3, 512, F32)
        vn_r = Rot(S, A, 3, 512, BF16)
        ya_r = Rot(S, A, 3, 512, F32)
        yag_r = Rot(S, A, 3, 512, BF16)
        yst_r = Rot(S, A, 3, 512, BF16)

        def a1_s1(i):
            hq, t = divmod(i, TT)
            zfill(2)
            preconv(i)
            if t == 0 and hq not in a1w:
                a1w[hq] = (load_w(hq * 512), load_w(1024 + hq * 512))
            if hq == 1 and t == 2:
                a2w[0] = (load_w(2048), load_w(3072))
            su, sv = a1w[hq]
            pu, pv = (i % 2) * 2, (i % 2) * 2 + 1
            for (pb, sl) in ((pu, su), (pv, sv)):
                for k in range(16):
                    S.op("pe", lambda e, pb=pb, sl=sl, k=k: e.matmul(
                        PM[pb][:, :], lhsT=xnT[:, k, t * 128:(t + 1) * 128], rhs=wslot[sl][:, k, :],
                        start=(k == 0), stop=(k == 15)),
                        reads=[xnTb, wslotb[sl]], writes=[PMb[pb]])
            gu, gub = gu_r.next()
            gv, gvb = gv_r.next()
            sq, sqb = sq_r.next()
            vn, vnb = vn_r.next()
            S.op("act", lambda e: e.activation(out=gv, in_=PM[pv][:, :], func=AF.Gelu_apprx_tanh),
                 reads=[PMb[pv]], writes=[gvb])
            S.op("act", lambda e: e.activation(out=gu, in_=PM[pu][:, :], func=AF.Gelu_apprx_tanh),
                 reads=[PMb[pu]], writes=[gub])
            S.op("dve", lambda e: e.tensor_tensor(out=sq, in0=gv, in1=gv, op=mult),
                 reads=[gvb], writes=[sqb])
            ssv, ssvb = st.next()
            S.op("dve", lambda e: e.tensor_reduce(
                out=ssv[:, 0:4], in_=sq.rearrange("p (h d) -> p h d", h=4), axis=AX.X, op=add),
                reads=[sqb], writes=[ssvb])
            rsv, rsvb = rstd(ssv[:, 0:4], ssvb, 4, 1.0 / 128)
            for h in range(4):
                hs = slice(h * 128, (h + 1) * 128)
                gs = slice(hq * 512 + h * 128, hq * 512 + (h + 1) * 128)
                S.op("dve", lambda e, hs=hs, gs=gs, h=h: e.scalar_tensor_tensor(
                    out=vn[:, hs], in0=gv[:, hs], scalar=rsv[:, h:h + 1], in1=gvn[:, gs],
                    op0=mult, op1=mult), reads=[gvb, rsvb, gvnb], writes=[vnb])
            return (i, hq, t, gu, gub, vn, vnb)

        def a1_s2(ctx):
            i, hq, t, gu, gub, vn, vnb = ctx
            ya, yab = ya_r.next()
            yag, yagb = yag_r.next()
            pmx = 4 + (i % 2)
            for h in range(4):
                hs = slice(h * 128, (h + 1) * 128)
                S.op("pe", lambda e, hs=hs, h=h: e.matmul(
                    PM[pmx][:, hs], lhsT=wsT[:, hq * 4 + h, :], rhs=vn[:, hs], start=True, stop=True),
                    reads=[wsTb, vnb], writes=[PMb[pmx]])
            for h in range(4):
                hs = slice(h * 128, (h + 1) * 128)
                hd = hq * 4 + h
                S.op("dve", lambda e, hs=hs, hd=hd: e.scalar_tensor_tensor(
                    out=ya[:, hs], in0=PM[pmx][:, hs], scalar=bsT[:, hd:hd + 1], in1=gu[:, hs],
                    op0=add, op1=mult), reads=[PMb[pmx], gub, bsTb], writes=[yab])
            c = t * 2 + hq
            S.op("act", lambda e: e.activation(out=junk[:, 0:512], in_=ya, func=AF.Square,
                                               accum_out=ssA[:, c:c + 1]),
                 reads=[yab], writes=[junkb, ssAb])
            S.op("dve", lambda e: e.tensor_tensor(
                out=yag, in0=ya, in1=ga[:, hq * 512:(hq + 1) * 512], op=mult),
                reads=[yab, gab], writes=[yagb])
            return (i, hq, t, yag, yagb)

        def a1_s3(ctx):
            i, hq, t, yag, yagb = ctx
            yst, ystb = yst_r.next()
            pt = i % 2
            for h in range(4):
                hs = slice(h * 128, (h + 1) * 128)
                S.op("pe", lambda e, hs=hs: e.transpose(
                    out=PT[pt][:, hs], in_=yag[:, hs], identity=ident),
                    reads=[yagb, identb], writes=[PTb[pt]])
            S.op("act", lambda e: e.copy(out=yst, in_=PT[pt][:, 0:512]),
                 reads=[PTb[pt]], writes=[ystb])
            dma("sp", yT_d[t, :, hq * 4:(hq + 1) * 4, :], yst.rearrange("p (k n) -> p k n", k=4),
                [ystb], [S.buf()])

        skew3(2 * TT, a1_s1, a1_s2, a1_s3)

        if phases == 1:
            finish()
            return nc
        S.barrier()
        A.off = markA
        cgs_r = Rot(S, A, 2, 512, F32)
        zb_r = Rot(S, A, 2, 514, F32)
        c1_r = Rot(S, A, 2, 512, F32)
        yb_r = Rot(S, A, 2, 512, F32)
        sqb_r = Rot(S, A, 2, 512, F32)
        ybs_r = Rot(S, A, 2, 512, BF16)
        hcs_r = Rot(S, A, 2, 8, F32)
        accB = A.alloc(NT, F32); accBb = S.buf()
        PH = PT[0][:, :].bitcast(F32)
        PHb = PTb[0]
        pset = 0
        for grp in range(2):
            if grp in a2w:
                sb_, sc_ = a2w[grp]
            else:
                sb_ = load_w(2048 + grp * 512)
                sc_ = load_w(3072 + grp * 512)
            sh_ = load_w(4096 + grp * 512)
            for c4 in range(4):
                cb = grp * 4 + c4
                cs = slice(c4 * 128, (c4 + 1) * 128)
                zprev = None
                for tg in range(4):
                    ts_ = slice(tg * 512, (tg + 1) * 512)
                    base = (pset % 2) * 3
                    pset += 1
                    pbg, pcg, phv = base, base + 1, base + 2
                    if tg == 0:
                        for (c0, sl) in ((0, sc_), (2, sh_)):
                            for k in range(16):
                                S.op("pe", lambda e, c0=c0, sl=sl, k=k, cs=cs: e.matmul(
                                    PH[:, c0:c0 + 2], lhsT=wslot[sl][:, k, cs], rhs=xnTh[:, k, :],
                                    start=(k == 0), stop=(k == 15)),
                                    reads=[wslotb[sl], xnThb], writes=[PHb], pe_acc=not (c0 == 0 and k == 0))
                    for (pb, sl) in ((pbg, sb_), (pcg, sc_), (phv, sh_)):
                        for k in range(16):
                            S.op("pe", lambda e, pb=pb, sl=sl, k=k, cs=cs, ts_=ts_: e.matmul(
                                PM[pb][:, :], lhsT=wslot[sl][:, k, cs], rhs=xnT[:, k, ts_],
                                start=(k == 0), stop=(k == 15)),
                                reads=[wslotb[sl], xnTb], writes=[PMb[pb]], pe_acc=(k > 0))
                    zfill(2)
                    preconv(32 + pset - 1)
                    cgs, cgsb = cgs_r.next()
                    zb, zbb = zb_r.next()
                    c1, c1b = c1_r.next()
                    yb, ybb = yb_r.next()
                    sqq, sqqb = sqb_r.next()
                    ybs, ybsb = ybs_r.next()
                    if tg == 0:
                        hcs, hcsb = hcs_r.next()
                        S.op("act", lambda e, hcs=hcs: e.copy(out=hcs[:, 0:2], in_=PH[:, 0:2]),
                             reads=[PHb], writes=[hcsb])
                        S.op("dve", lambda e, zb=zb, hcs=hcs: e.tensor_tensor(
                            out=zb[:, 0:2], in0=hcs[:, 0:2], in1=PH[:, 2:4], op=mult),
                            reads=[hcsb, PHb], writes=[zbb])
                    else:
                        zp, zpb = zprev
                        S.op("dve", lambda e, zb=zb, zp=zp: e.tensor_copy(out=zb[:, 0:2], in_=zp[:, 512:514]),
                             reads=[zpb], writes=[zbb])
                    S.op("act", lambda e, cgs=cgs, pcg=pcg: e.copy(out=cgs, in_=PM[pcg][:, :]),
                         reads=[PMb[pcg]], writes=[cgsb])
                    S.op("dve", lambda e, zb=zb, cgs=cgs, phv=phv: e.tensor_tensor(
                        out=zb[:, 2:514], in0=cgs, in1=PM[phv][:, :], op=mult),
                        reads=[cgsb, PMb[phv]], writes=[zbb])
                    S.op("dve", lambda e, c1=c1, zb=zb, cb=cb: e.tensor_scalar(
                        out=c1, in0=zb[:, 2:514], scalar1=cw[:, cb, 2:3], scalar2=None, op0=mult),
                        reads=[zbb, cwb], writes=[c1b])
                    S.op("dve", lambda e, c1=c1, zb=zb, cb=cb: e.scalar_tensor_tensor(
                        out=c1, in0=zb[:, 1:513], scalar=cw[:, cb, 1:2], in1=c1, op0=mult, op1=add),
                        reads=[zbb, cwb, c1b], writes=[c1b])
                    S.op("dve", lambda e, c1=c1, zb=zb, cb=cb: e.scalar_tensor_tensor(
                        out=c1, in0=zb[:, 0:512], scalar=cw[:, cb, 0:1], in1=c1, op0=mult, op1=add),
                        reads=[zbb, cwb, c1b], writes=[c1b])
                    S.op("dve", lambda e, yb=yb, c1=c1, pbg=pbg: e.tensor_tensor(
                        out=yb, in0=c1, in1=PM[pbg][:, :], op=mult),
                        reads=[c1b, PMb[pbg]], writes=[ybb])
                    S.op("act", lambda e, sqq=sqq, yb=yb: e.activation(out=sqq, in_=yb, func=AF.Square),
                         reads=[ybb], writes=[sqqb])
                    if cb == 0:
                        S.op("dve", lambda e, sqq=sqq, ts_=ts_: e.tensor_copy(out=accB[:, ts_], in_=sqq),
                             reads=[sqqb], writes=[accBb])
                    else:
                        S.op("dve", lambda e, sqq=sqq, ts_=ts_: e.tensor_tensor(
                            out=accB[:, ts_], in0=accB[:, ts_], in1=sqq, op=add),
                            reads=[sqqb, accBb], writes=[accBb])
                    S.op("act", lambda e, ybs=ybs, yb=yb, cb=cb: e.activation(
                        out=ybs, in_=yb, func=AF.Copy, scale=gbt[:, cb:cb + 1]),
                        reads=[ybb, gbtb], writes=[ybsb])
                    dma("sp", yT_d[tg * 4:(tg + 1) * 4, :, 8 + cb, :].rearrange("t p n -> p t n"),
                        ybs.rearrange("p (t n) -> p t n", t=4), [ybsb], [S.buf()])
                    zprev = (zb, zbb)

        ssa2, ssa2b = sm.next()
        S.op("dve", lambda e: e.tensor_reduce(out=ssa2[:, 0:16], in_=ssA.rearrange("p (t q) -> p t q", q=2),
                                              axis=AX.X, op=add), reads=[ssAb], writes=[ssa2b])
        r_, r_b = rstd(ssa2[:, 0:16], ssa2b, 16, 1.0 / 1024)
        S.op("dve", lambda e: e.tensor_copy(out=rsA, in_=r_[:, 0:16]), reads=[r_b], writes=[rsAb])
        for t in range(TT):
            S.op("pe", lambda e, t=t: e.matmul(PM[4][:, t:t + 1], lhsT=accB[:, t * 128:(t + 1) * 128],
                                                rhs=onesf[:, 0:1], start=True, stop=True),
                 reads=[accBb, onesfb], writes=[PMb[4]], pe_acc=(t > 0))
        ssb2, ssb2b = sm.next()
        S.op("dve", lambda e: e.tensor_copy(out=ssb2[:, 0:16], in_=PM[4][:, 0:16]), reads=[PMb[4]], writes=[ssb2b])
        r2_, r2_b = rstd(ssb2[:, 0:16], ssb2b, 16, 1.0 / 1024)
        S.op("dve", lambda e: e.tensor_copy(out=rsB, in_=r2_[:, 0:16]), reads=[r2_b], writes=[rsBb])

        if phases == 2:
            if debug:
                dma("sp", dbg2_d[:, 0:16], rsA, [rsAb], [S.buf()])
                dma("sp", dbg2_d[:, 16:32], rsB, [rsBb], [S.buf()])
            finish()
            return nc
        S.barrier()
        E0_WORDS = 2 * 16 * 512 // 2
        A.off = A.n - E0_WORDS
        wg0 = A.alloc3(16, 512, BF16); wg0b = S.bufs(4)
        wu0 = A.alloc3(16, 512, BF16); wu0b = S.bufs(4)
        A.off = 0
        A_lim = A.n - E0_WORDS
        wout = A.alloc3(16, D, BF16); woutb = S.bufs(4)
        wout_v = wout_d.rearrange("(k p) n -> p k n", p=128)
        for nb in range(4):
            ns = slice(nb * 512, (nb + 1) * 512)
            dma("pool", wout[:, :, ns], wout_v[:, :, ns], [], [woutb[nb]])
        dma("sp", gslot, gffn_d, [], [gslotb])

        def load_gu(dst, src, dstb):
            dflat = dst.rearrange("p k n -> p (k n)")
            sflat = src.rearrange("(p k) n -> p (k n)", k=16)
            for q in range(4):
                dma("pool", dflat[:, q * 2048:(q + 1) * 2048], sflat[:, q * 2048:(q + 1) * 2048], [], [dstb[q]])

        if phases >= 4:
            load_gu(wg0, wg_d[0], wg0b)
            load_gu(wu0, wu_d[0], wu0b)
        yt_r = Rot(S, A, 2, 2048, BF16, a=16)
        xt = Rot(S, A, 2, D, F32)
        ht_r = Rot(S, A, 3, D, F32)
        hn_r = Rot(S, A, 4, D, BF16)
        lg_r = Rot(S, A, 3, 40, F32)
        hnT_r = Rot(S, A, 2, 2048, BF16, a=16)
        tmp_r = Rot(S, A, 2, 512, F32)
        pab = [0]

        assert A.off <= A_lim, ("A3 arena overlaps E0", A.off, A_lim)
        a3ld = {}

        def a3_load(t):
            yt, ytb = yt_r.next()
            xtt, xttb = xt.next()
            dma("sp", yt, yT_d[t], [], [ytb])
            dma("sp", xtt, x_d[t * 128:(t + 1) * 128, :], [], [xttb])
            a3ld[t] = (yt, ytb, xtt, xttb)

        a3_load(0)

        def a3_s1(t):
            if t + 1 < TT:
                a3_load(t + 1)
            yt, ytb, xtt, xttb = a3ld.pop(t)
            ht, htb = ht_r.next()
            for nb in range(4):
                ns = slice(nb * 512, (nb + 1) * 512)
                pa, pb_ = (pab[0] % 2) * 2, (pab[0] % 2) * 2 + 1
                pab[0] += 1
                for (pp, k0) in ((pa, 0), (pb_, 8)):
                    for k in range(k0, k0 + 8):
                        S.op("pe", lambda e, pp=pp, k=k, ns=ns, k0=k0: e.matmul(
                            PM[pp][:, :], lhsT=yt[:, k, :], rhs=wout[:, k, ns],
                            start=(k == k0), stop=(k == k0 + 7)),
                            reads=[ytb, woutb[nb]], writes=[PMb[pp]])
                tmp, tmpb = tmp_r.next()
                hm, hmb = tmp, tmpb
                S.op("act", lambda e, tmp=tmp, pa=pa: e.activation(
                    out=tmp, in_=PM[pa][:, :], func=AF.Copy, scale=rsA[:, t:t + 1]),
                    reads=[PMb[pa], rsAb], writes=[tmpb])
                S.op("dve", lambda e, hm=hm, tmp=tmp, pb_=pb_: e.scalar_tensor_tensor(
                    out=hm, in0=PM[pb_][:, :], scalar=rsB[:, t:t + 1], in1=tmp, op0=mult, op1=add),
                    reads=[PMb[pb_], rsBb, tmpb], writes=[hmb])
                S.op("dve", lambda e, hm=hm, ns=ns: e.tensor_tensor(
                    out=ht[:, ns], in0=hm, in1=xtt[:, ns], op=add),
                    reads=[hmb, xttb], writes=[htb])
            dma("sp", h_d[t * 128:(t + 1) * 128, :], ht, [htb], [S.buf()])
            return (t, ht, htb)

        def a3_sB(ctx):
            t, ht, htb = ctx
            hn, hnb = hn_r.next()
            ss, ssb = st.next()
            S.op("act", lambda e: e.activation(out=junk, in_=ht, func=AF.Square, accum_out=ss[:, 0:1]),
                 reads=[htb], writes=[junkb, ssb])
            rs, rsb = rstd(ss[:, 0:1], ssb, 1, 1.0 / D)
            S.op("dve", lambda e: e.scalar_tensor_tensor(
                out=hn, in0=ht, scalar=rs[:, 0:1], in1=gslot, op0=mult, op1=mult),
                reads=[htb, rsb, gslotb], writes=[hnb])
            return (t, hn, hnb)

        def a3_s2(ctx):
            t, hn, hnb = ctx
            hnT, hnTb = hnT_r.next()
            for half in range(2):
                for kk in range(8):
                    k = half * 8 + kk
                    S.op("pe", lambda e, half=half, kk=kk, k=k: e.transpose(
                        out=PT[half][:, kk * 128:(kk + 1) * 128], in_=hn[:, k * 128:(k + 1) * 128],
                        identity=ident), reads=[hnb, identb], writes=[PTb[half]])
                srcp = PT[half][:, :].rearrange("p (k n) -> p k n", k=8)
                dst = hnT[:, half * 8:(half + 1) * 8, :]
                S.op("act", lambda e, dst=dst, srcp=srcp: e.copy(out=dst, in_=srcp),
                     reads=[PTb[half]], writes=[hnTb])
            for k in range(16):
                S.op("pe", lambda e, hnT=hnT, k=k: e.matmul(
                    PM[4][:, 0:36], lhsT=hnT[:, k, :], rhs=wr[:, k, :], start=(k == 0), stop=(k == 15)),
                    reads=[hnTb, wrb], writes=[PMb[4]], pe_acc=(k > 0))
            lg, lgb = lg_r.next()
            S.op("dve", lambda e, lg=lg: e.tensor_tensor(out=lg[:, 0:36], in0=PM[4][:, 0:36], in1=rb, op=add),
                 reads=[PMb[4], rbb], writes=[lgb])
            return (t, hn, hnb, lg, lgb)

        def a3_s2b(ctx):
            t, hn, hnb, lg, lgb = ctx
            s1, s1b = st.next()
            s2, s2b = st.next()
            wk, wkb = sm.next()
            lem, lemb = sm.next()
            mk1, mk1b = sm.next()
            mk2, mk2b = sm.next()
            V = lambda f, r, w: S.op("dve", f, reads=r, writes=w)
            V(lambda e, lg=lg, s1=s1: e.tensor_reduce(out=s1[:, 0:1], in_=lg[:, 0:4], axis=AX.X, op=ALU.max),
              [lgb], [s1b])
            V(lambda e, lg=lg, s1=s1, t=t: e.tensor_scalar(
                out=rstat[:, t, 0:4], in0=lg[:, 0:4], scalar1=s1[:, 0:1], scalar2=None, op0=ALU.subtract),
              [lgb, s1b], [rstatb[t]])
            V(lambda e, wk=wk, lg=lg, s1=s1: e.tensor_scalar(
                out=wk[:, 4:8], in0=lg[:, 0:4], scalar1=s1[:, 0:1], scalar2=None, op0=ALU.is_ge),
              [lgb, s1b, wkb], [wkb])
            V(lambda e, wk=wk: e.tensor_scalar(out=wk[:, 8:12], in0=wk[:, 4:8], scalar1=-1.0, scalar2=BIG,
                                               op0=add, op1=mult), [wkb], [wkb])
            for g in range(4):
                V(lambda e, lem=lem, lg=lg, wk=wk, g=g: e.tensor_scalar(
                    out=lem[:, g * 8:(g + 1) * 8], in0=lg[:, 4 + g * 8:4 + (g + 1) * 8],
                    scalar1=wk[:, 8 + g:9 + g], scalar2=None, op0=add), [lgb, wkb], [lemb])
            V(lambda e, lem=lem, s1=s1: e.tensor_reduce(out=s1[:, 4:5], in_=lem[:, 0:32], axis=AX.X, op=ALU.max),
              [lemb, s1b], [s1b])
            V(lambda e, mk1=mk1, lem=lem, s1=s1: e.tensor_scalar(
                out=mk1[:, 0:32], in0=lem[:, 0:32], scalar1=s1[:, 4:5], scalar2=None, op0=ALU.is_ge),
              [lemb, s1b], [mk1b])
            V(lambda e, lem=lem, mk1=mk1: e.scalar_tensor_tensor(
                out=lem[:, 0:32], in0=mk1[:, 0:32], scalar=-BIG, in1=lem[:, 0:32], op0=mult, op1=add),
              [mk1b, lemb], [lemb])
            V(lambda e, lem=lem, s1=s1: e.tensor_reduce(out=s1[:, 6:7], in_=lem[:, 0:32], axis=AX.X, op=ALU.max),
              [lemb, s1b], [s1b])
            V(lambda e, mk2=mk2, lem=lem, s1=s1: e.tensor_scalar(
                out=mk2[:, 0:32], in0=lem[:, 0:32], scalar1=s1[:, 6:7], scalar2=None, op0=ALU.is_ge),
              [lemb, s1b], [mk2b])
            V(lambda e, s1=s1, t=t: e.tensor_tensor(out=rstat[:, t, 4:5], in0=s1[:, 6:7], in1=s1[:, 4:5],
                                                    op=ALU.subtract), [s1b], [rstatb[t]])
            V(lambda e, mk1=mk1, mk2=mk2, t=t: e.tensor_tensor(
                out=Aall[:, t, :], in0=mk1[:, 0:32], in1=mk2[:, 0:32], op=add),
              [mk1b, mk2b], [Aallb[t]])
            return (t, hn, hnb, wk, wkb, lem, lemb, mk1, mk1b, mk2, mk2b, s2, s2b)

        def a3_s3(ctx):
            t, hn, hnb, wk, wkb, lem, lemb, mk1, mk1b, mk2, mk2b, s2, s2b = ctx
            V = lambda f, r, w: S.op("dve", f, reads=r, writes=w)
            for tp in range(t + 1):
                S.op("pe", lambda e, tp=tp, t=t: e.matmul(
                    PM[5][:, 0:32], lhsT=(ones if tp < t else utri), rhs=Aall[:, tp, :],
                    start=(tp == 0), stop=(tp == t)),
                    reads=[Aallb[tp], onesb, utrib], writes=[PMb[5]], pe_acc=(tp > 0))
            V(lambda e, wk=wk: e.tensor_tensor(out=wk[:, 0:32], in0=PM[5][:, 0:32], in1=iotc, op=add),
              [PMb[5], iotcb, wkb], [wkb])
            V(lambda e, lem=lem: e.tensor_scalar(out=lem[:, 0:32], in0=PM[5][:, 0:32], scalar1=CAP - 0.5,
                                                 scalar2=1.0e6, op0=ALU.is_ge, op1=mult),
              [PMb[5], lemb], [lemb])
            V(lambda e, wk=wk, lem=lem: e.tensor_tensor(out=wk[:, 0:32], in0=wk[:, 0:32], in1=lem[:, 0:32], op=add),
              [wkb, lemb], [wkb])
            V(lambda e, mk1=mk1, wk=wk: e.tensor_tensor(out=mk1[:, 0:32], in0=mk1[:, 0:32], in1=wk[:, 0:32], op=mult),
              [mk1b, wkb], [mk1b])
            V(lambda e, mk2=mk2, wk=wk: e.tensor_tensor(out=mk2[:, 0:32], in0=mk2[:, 0:32], in1=wk[:, 0:32], op=mult),
              [mk2b, wkb], [mk2b])
            V(lambda e, mk1=mk1, s2=s2: e.tensor_reduce(out=s2[:, 3:4], in_=mk1[:, 0:32], axis=AX.X, op=add),
              [mk1b, s2b], [s2b])
            V(lambda e, mk2=mk2, s2=s2: e.tensor_reduce(out=s2[:, 4:5], in_=mk2[:, 0:32], axis=AX.X, op=add),
              [mk2b, s2b], [s2b])
            V(lambda e, s2=s2, t=t: e.tensor_copy(out=idx[:, t, 0:2], in_=s2[:, 3:5]), [s2b], [idxb[t]])
            for j in range(2):
                S.dma("pool", lambda e, hn=hn, t=t, j=j: e.indirect_dma_start(
                    out=xs_d, out_offset=bass.IndirectOffsetOnAxis(ap=idx[:, t, j:j + 1], axis=0),
                    in_=hn, in_offset=None, bounds_check=bcreg[0], oob_is_err=False),
                    reads=[hnb, idxb[t]], writes=[S.buf()])

        cA, cB, cC = {}, {}, {}
        for c in range(TT + 3):
            if 0 <= c - 3 < TT:
                a3_s3(cC.pop(c - 3))
            if 0 <= c - 2 < TT:
                cC[c - 2] = a3_s2b(a3_s2(cB.pop(c - 2)))
            if 0 <= c - 1 < TT:
                cB[c - 1] = a3_sB(cA.pop(c - 1))
            if c < TT:
                cA[c] = a3_s1(c)

        ge, geb = sm.next()
        g2, g2b = sm.next()
        S.op("act", lambda e: e.activation(out=gex.rearrange("p (t k) -> p t k", k=5), in_=rstat[:, :, 0:5], func=AF.Exp),
             reads=rstatb, writes=[gexb])
        gx3 = gex.rearrange("p (t k) -> p t k", k=5)
        S.op("dve", lambda e: e.tensor_reduce(out=ge[:, 0:16], in_=gx3[:, :, 0:4], axis=AX.X, op=add),
             reads=[gexb], writes=[geb])
        S.op("dve", lambda e: e.reciprocal(out=ge[:, 0:16], in_=ge[:, 0:16]), reads=[geb], writes=[geb])
        S.op("dve", lambda e: e.tensor_scalar(out=g2[:, 0:16], in0=gx3[:, :, 4], scalar1=1.0, scalar2=None, op0=add),
             reads=[gexb], writes=[g2b])
        S.op("dve", lambda e: e.reciprocal(out=g2[:, 0:16], in_=g2[:, 0:16]), reads=[g2b], writes=[g2b])
        S.op("dve", lambda e: e.tensor_tensor(out=g2[:, 16:32], in0=gx3[:, :, 4], in1=g2[:, 0:16], op=mult),
             reads=[gexb, g2b], writes=[g2b])
        S.op("dve", lambda e: e.tensor_tensor(out=gates[:, :, 0], in0=g2[:, 0:16], in1=ge[:, 0:16], op=mult),
             reads=[g2b, geb], writes=gatesb)
        S.op("dve", lambda e: e.tensor_tensor(out=gates[:, :, 1], in0=g2[:, 16:32], in1=ge[:, 0:16], op=mult),
             reads=[g2b, geb], writes=gatesb)

        NA = CAP // 128
        for tp in range(TT):
            S.op("pe", lambda e, tp=tp: e.matmul(PM[5][:, 0:32], lhsT=ones, rhs=Aall[:, tp, :],
                                                 start=(tp == 0), stop=(tp == TT - 1)),
                 reads=[Aallb[tp], onesb], writes=[PMb[5]])
        cnt, cntb = sm.next()
        S.op("dve", lambda e: e.tensor_copy(out=cnt[:, 0:32], in_=PM[5][:, 0:32]), reads=[PMb[5]], writes=[cntb])
        S.op("pool", lambda e: e.iota(rowid, pattern=[[CAP, NE], [128, NA]], base=0, channel_multiplier=1,
                                      allow_small_or_imprecise_dtypes=True), writes=[rowidb])
        S.op("pool", lambda e: e.iota(spos[:, 0:NA], pattern=[[128, NA]], base=0, channel_multiplier=1,
                                      allow_small_or_imprecise_dtypes=True), writes=[sposb])
        for a in range(NA):
            vm, vmb = sm.next()
            vt, vtb = sm.next()
            S.op("dve", lambda e, a=a, vm=vm: e.tensor_scalar(
                out=vm[:, 0:32], in0=cnt[:, 0:32], scalar1=spos[:, a:a + 1], scalar2=None, op0=ALU.is_gt),
                reads=[cntb, sposb], writes=[vmb])
            S.op("dve", lambda e, a=a, vt=vt: e.tensor_scalar(
                out=vt[:, 0:32], in0=rowid[:, :, a], scalar1=-1.0e6, scalar2=None, op0=add),
                reads=[rowidb], writes=[vtb])
            S.op("dve", lambda e, vt=vt, vm=vm: e.tensor_tensor(out=vt[:, 0:32], in0=vt[:, 0:32], in1=vm[:, 0:32], op=mult),
                 reads=[vtb, vmb], writes=[vtb])
            S.op("dve", lambda e, a=a, vt=vt: e.tensor_scalar(
                out=vidx[:, :, a], in0=vt[:, 0:32], scalar1=1.0e6, scalar2=None, op0=add),
                reads=[vtb], writes=[vidxb])
        if debug:
            dma("sp", dbg2_d[:, 32:64], gates.rearrange("p t j -> p (t j)"), gatesb, [S.buf()])
            dma("sp", dbg2_d[:, 64:96].bitcast(I32), idx.rearrange("p t j -> p (t j)"), idxb, [S.buf()])
        if phases == 3:
            finish()
            return nc
        S.barrier()
        A.off = 0
        wgs = [wg0, A.alloc3(16, 512, BF16)]; wgsb = [wg0b, S.bufs(4)]
        wus = [wu0, A.alloc3(16, 512, BF16)]; wusb = [wu0b, S.bufs(4)]
        wds = [A.alloc3(4, D, BF16) for _ in range(2)]; wdsb = [S.bufs(4) for _ in range(2)]
        xg_r = Rot(S, A, 2, NA * D, BF16, a=NA)
        for xg_ in xg_r.t:
            S.op("dve", lambda e, xg_=xg_: e.memset(xg_, 0.0), writes=[xg_r.b[xg_r.t.index(xg_)]])
        xT_r = Rot(S, A, 1, 16 * CAP, BF16, a=16)
        hT_r = Rot(S, A, 2, 4 * CAP, BF16, a=4)
        sg_r = Rot(S, A, 2, CAP, F32)
        yo_r = Rot(S, A, 2, D, BF16)
        assert A.off <= A_lim, ("M arena overlaps E0", A.off, A_lim)

        def load_expert(e_):
            s = e_ % 2
            if 1 <= e_ <= NPRE:
                j = e_ - 1
                for (dst, srcm, dstb) in ((wgs[s], wgb_d[j], wgsb[s]), (wus[s], wub_d[j], wusb[s])):
                    dma("sp", dst.rearrange("p k n -> p (k n)"), srcm.rearrange("(p k) n -> p (k n)", k=16), [], dstb)
                dsrc = wdb_d[j]
            else:
                gsrc, usrc, dsrc = wg_d[e_], wu_d[e_], wd_d[e_]
                if e_ > 0:
                    load_gu(wgs[s], gsrc, wgsb[s])
                    load_gu(wus[s], usrc, wusb[s])
            for k in range(4):
                dma("sp" if 1 <= e_ <= NPRE else "pool", wds[s][:, k, :], dsrc[k * 128:(k + 1) * 128, :], [], [wdsb[s][k]])

        xgs = {}

        def gather_expert(e_):
            xg, xgb = xg_r.next()
            xgs[e_] = (xg, xgb)
            for a in range(NA):
                S.dma("pool", lambda e, xg=xg, a=a, e_=e_: e.indirect_dma_start(
                    out=xg[:, a, :], out_offset=None, in_=xs_d,
                    in_offset=bass.IndirectOffsetOnAxis(ap=vidx[:, e_, a:a + 1], axis=0),
                    bounds_check=bcreg[0], oob_is_err=False), reads=[vidxb], writes=[xgb])

        load_expert(0)
        gather_expert(0)
        gu_i = 0
        py_i = 0
        for e_ in range(NE):
            if e_ + 1 < NE:
                load_expert(e_ + 1)
                gather_expert(e_ + 1)
            s = e_ % 2
            xg, xgb = xgs.pop(e_)
            xT, xTb = xT_r.next()
            hT, hTb = hT_r.next()
            for a in range(NA):
                for half in range(2):
                    for kk in range(8):
                        k = half * 8 + kk
                        S.op("pe", lambda e, xg=xg, a=a, half=half, kk=kk, k=k: e.transpose(
                            out=PT[half][:, kk * 128:(kk + 1) * 128], in_=xg[:, a, k::16],
                            identity=ident), reads=[xgb, identb], writes=[PTb[half]])
                    srcp = PT[half][:, :].rearrange("p (k n) -> p k n", k=8)
                    dst = xT[:, half * 8:(half + 1) * 8, a * 128:(a + 1) * 128]
                    if half == 0:
                        S.op("act", lambda e, dst=dst, srcp=srcp: e.copy(out=dst, in_=srcp),
                             reads=[PTb[half]], writes=[xTb])
                    else:
                        S.op("dve", lambda e, dst=dst, srcp=srcp: e.tensor_copy(out=dst, in_=srcp),
                             reads=[PTb[half]], writes=[xTb])
            for m in range(4):
                ms = slice(m * 128, (m + 1) * 128)
                pg_ = (gu_i % 2) * 2
                pu_ = pg_ + 1
                gu_i += 1
                for (pp, wsl, wslb) in ((pg_, wgs[s], wgsb[s]), (pu_, wus[s], wusb[s])):
                    for k in range(16):
                        S.op("pe", lambda e, pp=pp, wsl=wsl, k=k, ms=ms, xT=xT: e.matmul(
                            PM[pp][:, 0:CAP], lhsT=wsl[:, k, ms], rhs=xT[:, k, :],
                            start=(k == 0), stop=(k == 15)),
                            reads=[wslb[k // 4], xTb], writes=[PMb[pp]])
                sg, sgb = sg_r.next()
                S.op("act", lambda e, sg=sg, pg_=pg_: e.activation(out=sg, in_=PM[pg_][:, 0:CAP], func=AF.Silu),
                     reads=[PMb[pg_]], writes=[sgb])
                S.op("dve", lambda e, hT=hT, m=m, sg=sg, pu_=pu_: e.tensor_tensor(
                    out=hT[:, m, :], in0=sg, in1=PM[pu_][:, 0:CAP], op=mult),
                    reads=[sgb, PMb[pu_]], writes=[hTb])
            for a in range(NA):
                yo, yob = yo_r.next()
                for nb in range(4):
                    ns = slice(nb * 512, (nb + 1) * 512)
                    py = 4 + (py_i % 2)
                    py_i += 1
                    for m in range(4):
                        S.op("pe", lambda e, py=py, hT=hT, m=m, a=a, ns=ns, s=s: e.matmul(
                            PM[py][:, :], lhsT=hT[:, m, a * 128:(a + 1) * 128], rhs=wds[s][:, m, ns],
                            start=(m == 0), stop=(m == 3)),
                            reads=[hTb, wdsb[s][m]], writes=[PMb[py]])
                    if nb % 2 == 0:
                        S.op("act", lambda e, yo=yo, ns=ns, py=py: e.copy(out=yo[:, ns], in_=PM[py][:, :]),
                             reads=[PMb[py]], writes=[yob])
                    else:
                        S.op("dve", lambda e, yo=yo, ns=ns, py=py: e.tensor_copy(out=yo[:, ns], in_=PM[py][:, :]),
                             reads=[PMb[py]], writes=[yob])
                S.dma("pool", lambda e, yo=yo, a=a, e_=e_: e.indirect_dma_start(
                    out=Y_d, out_offset=bass.IndirectOffsetOnAxis(ap=vidx[:, e_, a:a + 1], axis=0),
                    in_=yo, in_offset=None, bounds_check=bcreg[0], oob_is_err=False),
                    reads=[yob, vidxb], writes=[S.buf()])
        if phases == 4:
            finish()
            return nc
        S.barrier()
        A.off = 0
        dma("sp", gslot, gfin_d, [], [gslotb])
        hc_r = Rot(S, A, 4, D, F32)
        y1_r = Rot(S, A, 3, D, BF16)
        y2_r = Rot(S, A, 3, D, BF16)
        ot_r = Rot(S, A, 2, D, F32)
        outbs = []
        cld = {}

        def c_load(t):
            hc, hcb = hc_r.next()
            y1, y1b = y1_r.next()
            y2, y2b = y2_r.next()
            dma("sp", hc, h_d[t * 128:(t + 1) * 128, :], [], [hcb])
            for (j, yy, yyb) in ((0, y1, y1b), (1, y2, y2b)):
                S.dma("pool", lambda e, yy=yy, t=t, j=j: e.indirect_dma_start(
                    out=yy, out_offset=None, in_=Y_d,
                    in_offset=bass.IndirectOffsetOnAxis(ap=idx[:, t, j:j + 1], axis=0),
                    bounds_check=bcreg[0], oob_is_err=False),
                    reads=[idxb[t]], writes=[yyb])
            cld[t] = (hc, hcb, y1, y1b, y2, y2b)

        c_load(0)
        c_load(1)

        def c_x(t):
            hc, hcb, y1, y1b, y2, y2b = cld.pop(t)
            S.op("dve", lambda e: e.scalar_tensor_tensor(
                out=hc, in0=y1, scalar=gates[:, t, 0:1], in1=hc, op0=mult, op1=add),
                reads=[y1b, gatesb[t], hcb], writes=[hcb])
            S.op("dve", lambda e: e.scalar_tensor_tensor(
                out=hc, in0=y2, scalar=gates[:, t, 1:2], in1=hc, op0=mult, op1=add),
                reads=[y2b, gatesb[t], hcb], writes=[hcb])
            ss, ssb = st.next()
            sd, sdb = sm.next()
            S.op("act", lambda e: e.activation(out=junk, in_=hc, func=AF.Square, accum_out=ss[:, 0:1]),
                 reads=[hcb], writes=[junkb, ssb])
            S.op("act", lambda e: e.activation(out=sd[:, 0:1], in_=ss[:, 0:1], func=AF.Sqrt,
                                               scale=1.0 / D, bias=epsb[:, 0:1]),
                 reads=[ssb, epsbb], writes=[sdb])
            return (t, hc, hcb, sd, sdb)

        def c_y(ctx):
            t, hc, hcb, sd, sdb = ctx
            ot, otb = ot_r.next()
            rs, rsb = sm.next()
            S.op("dve", lambda e: e.reciprocal(out=rs[:, 0:1], in_=sd[:, 0:1]), reads=[sdb], writes=[rsb])
            S.op("dve", lambda e: e.scalar_tensor_tensor(
                out=ot, in0=hc, scalar=rs[:, 0:1], in1=gslot, op0=mult, op1=mult),
                reads=[hcb, rsb, gslotb], writes=[otb])
            ob_ = S.buf()
            dma("sp", out_d[t * 128:(t + 1) * 128, :], ot, [otb], [ob_])
            outbs.append(ob_)

        cprev = None
        for t in range(TT):
            if t + 2 < TT:
                c_load(t + 2)
            cur = c_x(t)
            if cprev is not None:
                c_y(cprev)
            cprev = cur
        c_y(cprev)
        S.op("sp", lambda e: None, reads=outbs)
        S.barrier()
        S.emit()
    return nc


_NC_CACHE = {}


def _prep_inputs(inp):
    f = lambda a: np.ascontiguousarray(np.asarray(a, dtype=np.float32))
    bc = lambda v: np.ascontiguousarray(np.broadcast_to(np.asarray(v, np.float32).reshape(1, -1), (128, v.size)))
    x = np.asarray(inp["x"], np.float32)
    shared = {
        "gmix": bc(np.asarray(inp["norm_mix_g"])[0]),
        "gffn": bc(np.asarray(inp["norm_ffn_g"])[0]),
        "gfin": bc(np.asarray(inp["norm_final_g"])),
        "gvn": bc(np.asarray(inp["gmlp_v_norm_g"])[0]),
        "ga": bc(np.asarray(inp["out_norm_gmlp_g"])[0]),
        "gb": f(np.asarray(inp["out_norm_conv_g"])[0].reshape(8, 128).T),
        "wsT": f(np.asarray(inp["gmlp_ws"])[0].transpose(2, 0, 1)),
        "bsT": f(np.asarray(inp["gmlp_bs"])[0].T),
        "cw": f(np.asarray(inp["conv_w"])[0].reshape(3, 8, 128).transpose(2, 1, 0)),
        "w_in": f(np.asarray(inp["w_in"])[0]),
        "w_out": f(np.asarray(inp["w_out"])[0]),
        "wr": f(np.concatenate([np.asarray(inp["router_group_w"])[0], np.asarray(inp["router_expert_w"])[0]],
                               axis=1).reshape(16, 128, 36).transpose(1, 0, 2)),
        "rb": bc(np.concatenate([np.asarray(inp["router_group_b"])[0], np.asarray(inp["router_expert_b"])[0]])),
        "wg": f(np.asarray(inp["expert_w_gate"])[0]),
        "wu": f(np.asarray(inp["expert_w_up"])[0]),
        "wd": f(np.asarray(inp["expert_w_down"])[0]),
    }
    in_maps = []
    for c in range(8):
        b, half = c // 2, c % 2
        s0 = half * NT
        m = dict(shared)
        m["x"] = np.ascontiguousarray(x[b, s0:s0 + NT])
        m["xh"] = np.ascontiguousarray(x[b, s0 - 2:s0]) if half else np.zeros((2, D), np.float32)
        in_maps.append(m)
    return in_maps


def kernel(**inputs):
    in_maps = _prep_inputs(inputs)
    if "nc" not in _NC_CACHE:
        _NC_CACHE["nc"] = build_nc()
    nc = _NC_CACHE["nc"]
    res = run_bass_kernel_spmd(nc, in_maps, core_ids=list(range(8)))
    out = np.empty((4, 4096, D), np.float32)
    for c in range(8):
        b, half = c // 2, c % 2
        out[b, half * NT:(half + 1) * NT] = res.results[c]["out"]
    return out
```

```python
import numpy as np
import concourse.bass as bass
import concourse.mybir as mybir
from contextlib import ExitStack

F32 = mybir.dt.float32
BF16 = mybir.dt.bfloat16
I32 = mybir.dt.int32
U32 = mybir.dt.uint32
AF = mybir.ActivationFunctionType
ALU = mybir.AluOpType
AX = mybir.AxisListType

ENGS = ("pe", "act", "dve", "pool", "sp")
NDMASEM = {"sp": 12, "act": 8, "pool": 20}


class Buf:
    __slots__ = ("name", "w", "r")

    def __init__(self, name=""):
        self.name = name
        self.w = None
        self.r = []


class Sched:
    def __init__(self, nc, es):
        self.nc = nc
        self.es = es
        self.ops = {e: [] for e in ENGS}
        self.ndma = {q: 0 for q in NDMASEM}
        self.nsb = 0

    def buf(self, name=""):
        return Buf(name)

    def bufs(self, n, name=""):
        return [Buf(f"{name}{i}") for i in range(n)]

    def sb(self, shape, dtype, name=None):
        self.nsb += 1
        return self.es.enter_context(self.nc.sbuf_tensor(name or f"sb{self.nsb}", list(shape), dtype))

    def ps(self, shape, dtype, name=None):
        self.nsb += 1
        return self.es.enter_context(self.nc.psum_tensor(name or f"ps{self.nsb}", list(shape), dtype))

    def _deps(self, reads, writes):
        deps = []
        for b in reads:
            if b.w is not None:
                deps.append(b.w)
        for b in writes:
            if b.w is not None:
                deps.append(b.w)
            deps.extend(b.r)
        return deps

    def _commit(self, ev, reads, writes):
        for b in reads:
            b.r.append(ev)
        for b in writes:
            b.w = ev
            b.r = []

    def op(self, eng, fn, reads=(), writes=(), pe_acc=False):
        deps = self._deps(reads, writes)
        if pe_acc:
            deps = [d for d in deps if not (d[0] == "c" and d[1] == "pe")]
        idx = len(self.ops[eng])
        ev = ("c", eng, idx)
        self.ops[eng].append(("c", fn, deps, None))
        self._commit(ev, reads, writes)
        return ev

    def dma(self, q, fn, reads=(), writes=()):
        deps = self._deps(reads, writes)
        j = self.ndma[q]
        self.ndma[q] += 1
        K = NDMASEM[q]
        if j >= K:
            deps.append(("d", q, j - K))
        idx = len(self.ops[q])
        ev = ("d", q, j)
        self.ops[q].append(("d", fn, deps, j))
        self._commit(ev, reads, writes)
        return ev

    def emit(self, final_events=()):
        nc = self.nc
        es = self.es
        csem = {e: es.enter_context(nc.semaphore(f"c_{e}")) for e in ("pe", "act", "dve", "pool")}
        dsem = {q: [es.enter_context(nc.semaphore(f"d_{q}{i}")) for i in range(K)] for q, K in NDMASEM.items()}
        waits = {e: [] for e in ENGS}
        signal = {e: set() for e in ("pe", "act", "dve", "pool")}
        for e in ENGS:
            seen_c = {}
            seen_d = {}
            for (kind, fn, deps, j) in self.ops[e]:
                wc = {}
                wd = {}
                for d in deps:
                    if d[0] == "c":
                        if d[1] == "pe" and e == "pe":
                            continue
                        wc[d[1]] = max(wc.get(d[1], -1), d[2])
                    else:
                        q, jj = d[1], d[2]
                        K = NDMASEM[q]
                        key = (q, jj % K)
                        wd[key] = max(wd.get(key, -1), jj // K)
                w = []
                for x, i in wc.items():
                    if seen_c.get(x, -1) >= i:
                        continue
                    seen_c[x] = i
                    signal[x].add(i)
                    w.append(("c", x, i))
                for key, u in wd.items():
                    if seen_d.get(key, -1) >= u:
                        continue
                    seen_d[key] = u
                    w.append(("d", key[0], key[1], u))
                waits[e].append(w)
        cnt = {}
        for x in signal:
            s = sorted(signal[x])
            cnt[x] = {i: k + 1 for k, i in enumerate(s)}
        engobj = {"pe": nc.tensor, "act": nc.scalar, "dve": nc.vector, "pool": nc.gpsimd, "sp": nc.sync}
        self.stats = {e: len(self.ops[e]) for e in ENGS}

        def run2(e, eng):
            for idx, ((kind, fn, deps, j), w) in enumerate(zip(self.ops[e], waits[e])):
                for ww in w:
                    if ww[0] == "c":
                        eng.wait_ge(csem[ww[1]], cnt[ww[1]][ww[2]])
                    else:
                        eng.wait_ge(dsem[ww[1]][ww[2]], 16 * (ww[3] + 1))
                inst = fn(eng)
                if kind == "d":
                    K = NDMASEM[e]
                    inst.then_inc(dsem[e][j % K], 16)
                elif inst is not None and idx in signal.get(e, ()):
                    inst.then_inc(csem[e], 1)

        with nc.Block() as block:
            @block.tensor
            def _(eng):
                run2("pe", eng)

            @block.scalar
            def _(eng):
                run2("act", eng)

            @block.vector
            def _(eng):
                run2("dve", eng)

            @block.gpsimd
            def _(eng):
                run2("pool", eng)

            @block.sync
            def _(eng):
                run2("sp", eng)


def _barrier(self):
    evs = []
    for e in ("pe", "act", "dve", "pool"):
        ops = self.ops[e]
        for i in range(len(ops) - 1, -1, -1):
            if ops[i][0] == "c" and ops[i][3] != "nop":
                evs.append(("c", e, i))
                break
    for q, K in NDMASEM.items():
        n = self.ndma[q]
        for j in range(max(0, n - K), n):
            evs.append(("d", q, j))
    for e in ENGS:
        self.ops[e].append(("c", (lambda eng: None), list(evs), "nop"))


Sched.barrier = _barrier

_DSZ = {F32: 4, BF16: 2, I32: 4, U32: 4}


class Arena:
    def __init__(self, S, nwords, name="arena"):
        self.t = S.sb([128, nwords], F32, name)
        self.n = nwords
        self.off = 0

    def alloc(self, free_elems, dtype):
        nw = (free_elems * _DSZ[dtype] + 3) // 4
        nw = (nw + 7) // 8 * 8
        assert self.off + nw <= self.n, ("arena overflow", self.off, nw, self.n)
        v = self.t[:, self.off:self.off + nw]
        self.off += nw
        if dtype != F32:
            v = v.bitcast(dtype)
        if v.shape[1] != free_elems:
            v = v[:, 0:free_elems]
        return v

    def alloc3(self, a, b, dtype):
        return self.alloc(a * b, dtype).rearrange("p (a b) -> p a b", a=a)


class Rot:
    def __init__(self, S, arena, n, free_elems, dtype, a=None):
        if a is None:
            self.t = [arena.alloc(free_elems, dtype) for _ in range(n)]
        else:
            self.t = [arena.alloc3(a, free_elems // a, dtype) for _ in range(n)]
        self.b = S.bufs(n)
        self.n = n
        self.i = -1

    def next(self):
        self.i += 1
        j = self.i % self.n
        return self.t[j], self.b[j]

from concourse.bass_utils import run_bass_kernel_spmd

D = 2048
NT = 2048
TT = 16
CAP = 384
NE = 32
EPS = 1e-6
BIG = 1.0e4
PHASES = 99
N_PRE = 4


def build_nc(phases=PHASES, debug=False):
    nc = bass.Bass("TRN2", target_bir_lowering=False)

    def din(name, shape, dt=F32):
        return nc.dram_tensor(name, list(shape), dt, kind="ExternalInput").ap()

    x_d = din("x", [NT, D])
    xh_d = din("xh", [2, D])
    gmix_d = din("gmix", [128, D])
    gffn_d = din("gffn", [128, D])
    gfin_d = din("gfin", [128, D])
    gvn_d = din("gvn", [128, 1024])
    ga_d = din("ga", [128, 1024])
    gb_d = din("gb", [128, 8])
    wsT_d = din("wsT", [128, 8, 128])
    bsT_d = din("bsT", [128, 8])
    cw_d = din("cw", [128, 8, 3])
    win_d = din("w_in", [D, 5120])
    wout_d = din("w_out", [D, D])
    wr_d = din("wr", [128, 16, 36])
    rb_d = din("rb", [128, 36])
    if phases >= 4:
        wg_d = din("wg", [NE, D, 512])
        wu_d = din("wu", [NE, D, 512])
        wd_d = din("wd", [NE, 512, D])
    IK = "ExternalOutput" if debug else "Internal"
    if debug:
        dbg_d = nc.dram_tensor("dbg", [128, 16 * NT], BF16, kind=("ExternalOutput" if phases == 0 else "Internal")).ap()
        dbg2_d = nc.dram_tensor("dbg2", [128, 160], F32, kind="ExternalOutput").ap()
    out_d = nc.dram_tensor("out", [NT, D], F32, kind="ExternalOutput").ap()
    yT_d = nc.dram_tensor("yT_s", [TT, 128, 16, 128], BF16, kind=(IK if phases in (1, 2) else "Internal")).ap()
    h_d = nc.dram_tensor("h_s", [NT, D], F32, kind=(IK if phases == 3 else "Internal")).ap()
    xs_d = nc.dram_tensor("xs_s", [NE * CAP, D], BF16, kind=(IK if phases == 3 else "Internal")).ap()
    Y_d = nc.dram_tensor("Y_s", [NE * CAP, D], BF16, kind=(IK if phases == 4 else "Internal")).ap()

    NPRE = N_PRE if phases >= 4 else 0
    if NPRE:
        wgb_d = nc.dram_tensor("wgb_s", [NPRE, D, 512], BF16, kind="Internal").ap()
        wub_d = nc.dram_tensor("wub_s", [NPRE, D, 512], BF16, kind="Internal").ap()
        wdb_d = nc.dram_tensor("wdb_s", [NPRE, 512, D], BF16, kind="Internal").ap()

    with ExitStack() as es:
        S = Sched(nc, es)
        mult, add = ALU.mult, ALU.add

        P = Arena(S, 8700, "persist")
        ident = P.alloc(128, BF16); identb = S.buf()
        utri = P.alloc(128, BF16); utrib = S.buf()
        ones = P.alloc(128, BF16); onesb = S.buf()
        onesf = P.alloc(8, F32); onesfb = S.buf()
        iotc = P.alloc(32, F32); iotcb = S.buf()
        gslot = P.alloc(D, F32); gslotb = S.buf()
        gvn = P.alloc(1024, F32); gvnb = S.buf()
        ga = P.alloc(1024, F32); gab = S.buf()
        gbt = P.alloc(8, F32); gbtb = S.buf()
        wsT = P.alloc3(8, 128, BF16); wsTb = S.buf()
        bsT = P.alloc(8, F32); bsTb = S.buf()
        cw = P.alloc3(8, 3, F32); cwb = S.buf()
        wr = P.alloc3(16, 36, BF16); wrb = S.buf()
        rb = P.alloc(36, F32); rbb = S.buf()
        ssA = P.alloc(32, F32); ssAb = S.buf()
        rsA = P.alloc(16, F32); rsAb = S.buf()
        rsB = P.alloc(16, F32); rsBb = S.buf()
        gates = P.alloc3(16, 2, F32); gatesb = S.bufs(16)
        idx = P.alloc3(16, 2, I32); idxb = S.bufs(16)
        Aall = P.alloc3(16, 32, BF16); Aallb = S.bufs(16)
        xnTh = P.alloc3(16, 2, BF16); xnThb = S.buf()
        junk = P.alloc(D, BF16); junkb = S.buf()
        rstat = P.alloc3(16, 5, F32); rstatb = S.bufs(16)
        gex = P.alloc(80, F32); gexb = S.buf()
        rowid = P.alloc3(NE, CAP // 128, F32); rowidb = S.buf()
        spos = P.alloc(8, F32); sposb = S.buf()
        vidx = P.alloc3(NE, CAP // 128, I32); vidxb = S.buf()
        st = Rot(S, P, 16, 8, F32)
        sm = Rot(S, P, 26, 40, F32)

        PT = [S.ps([128, 1024], BF16, f"pt{i}") for i in range(2)]
        PTb = S.bufs(2)
        PM = [S.ps([128, 512], F32, f"pm{i}") for i in range(6)]
        PMb = S.bufs(6)

        def dma(q, out, in_, reads, writes):
            S.dma(q, lambda e: e.dma_start(out=out, in_=in_), reads=reads, writes=writes)

        def finish():
            S.barrier()
            S.emit()

        bcreg = [None]

        def _mkreg(e):
            bcreg[0] = e.to_reg(NE * CAP - 1)
            return None
        S.op("pool", _mkreg)
        S.op("pool", lambda e: e.memset(ident, 0.0), writes=[identb])
        S.op("pool", lambda e: e.affine_select(out=ident, in_=ident, pattern=[[-1, 128]],
                                               compare_op=ALU.not_equal, fill=1.0, base=0,
                                               channel_multiplier=1), reads=[identb], writes=[identb])
        S.op("pool", lambda e: e.memset(ones, 1.0), writes=[onesb])
        S.op("pool", lambda e: e.memset(onesf, 1.0), writes=[onesfb])
        S.op("pool", lambda e: e.memset(utri, 1.0), writes=[utrib])
        S.op("pool", lambda e: e.affine_select(out=utri, in_=utri, pattern=[[1, 128]],
                                               compare_op=ALU.is_gt, fill=0.0, base=0,
                                               channel_multiplier=-1), reads=[utrib], writes=[utrib])
        S.op("pool", lambda e: e.iota(iotc, pattern=[[CAP, 32]], base=0, channel_multiplier=0,
                                      allow_small_or_imprecise_dtypes=True), writes=[iotcb])
        dma("sp", gslot, gmix_d, [], [gslotb])
        dma("sp", gvn, gvn_d, [], [gvnb])
        dma("sp", ga, ga_d, [], [gab])
        dma("sp", gbt, gb_d, [], [gbtb])
        dma("sp", bsT, bsT_d, [], [bsTb])
        dma("sp", cw, cw_d, [], [cwb])
        dma("sp", rb, rb_d, [], [rbb])
        dma("pool", wsT, wsT_d, [], [wsTb])
        dma("pool", wr, wr_d, [], [wrb])
        S.op("dve", lambda e: e.memset(wsT[64:128, :, 0:64], 0.0), reads=[wsTb], writes=[wsTb])

        A = Arena(S, 44200, "arena")
        xnT = A.alloc3(16, NT, BF16); xnTb = S.buf()
        wslot = [A.alloc3(16, 512, BF16) for _ in range(4)]; wslotb = S.bufs(4)
        zt = A.alloc3(2, D, BF16); ztb = S.buf()
        S.op("dve", lambda e: e.memset(zt, 0.0), writes=[ztb])
        NZ = NE * CAP // 256
        zfill_i = [0]

        def zfill(n):
            for _ in range(n):
                zi = zfill_i[0]
                zfill_i[0] += 1
                if zi < NZ:
                    dst = xs_d[zi * 256:(zi + 1) * 256, :]
                elif zi < 2 * NZ:
                    dst = Y_d[(zi - NZ) * 256:(zi - NZ + 1) * 256, :]
                else:
                    return
                dma("sp", dst.rearrange("(p a) d -> p a d", a=2), zt, [ztb], [S.buf()])

        prepend = []
        for j in range(NPRE):
            e_ = j + 1
            for (dstm, srcm) in ((wgb_d[j], wg_d[e_]), (wub_d[j], wu_d[e_])):
                dfl = dstm.rearrange("(p k) n -> p (k n)", k=16)
                sfl = srcm.rearrange("(p k) n -> p (k n)", k=16)
                for q in range(4):
                    prepend.append((dfl[:, q * 2048:(q + 1) * 2048], sfl[:, q * 2048:(q + 1) * 2048]))
            for k in range(4):
                prepend.append((wdb_d[j, k * 128:(k + 1) * 128, :], wd_d[e_, k * 128:(k + 1) * 128, :]))
        pre_i = [0]

        pre_tot = [None]

        def preconv_n(n):
            for _ in range(n):
                if prepend:
                    dst, src = prepend.pop(0)
                    dma("pool", dst, src, [], [S.buf()])

        def preconv(item, nitems=64):
            if pre_tot[0] is None:
                pre_tot[0] = len(prepend)
            while prepend and pre_i[0] * nitems < (item + 1) * pre_tot[0]:
                dst, src = prepend.pop(0)
                pre_i[0] += 1
                dma("pool", dst, src, [], [S.buf()])

        markA = A.off
        win_v = win_d.rearrange("(k p) n -> p k n", p=128)

        def rstd(ss_ap, ssb, n, scale, rows=128):
            sd, sdb = sm.next()
            rs, rsb = sm.next()
            S.op("act", lambda e: e.activation(out=sd[:rows, 0:n], in_=ss_ap, func=AF.Sqrt,
                                               scale=scale, bias=epsb[:rows, 0:1]),
                 reads=[ssb, epsbb], writes=[sdb])
            S.op("dve", lambda e: e.reciprocal(out=rs[:rows, 0:n], in_=sd[:rows, 0:n]),
                 reads=[sdb], writes=[rsb])
            return rs, rsb

        epsb = P.alloc(8, F32); epsbb = S.buf()
        S.op("pool", lambda e: e.memset(epsb, EPS), writes=[epsbb])

        nload = [0]
        NWS = len(wslot)

        def load_w(col0):
            s = nload[0] % NWS
            nload[0] += 1
            dma("pool", wslot[s], win_v[:, :, col0:col0 + 512], [], [wslotb[s]])
            return s

        a1w = {0: (load_w(0), load_w(1024))}
        a2w = {}

        xt = Rot(S, A, 3, D, F32)
        xn = Rot(S, A, 3, D, BF16)

        def a0_s1(t):
            rows = 128 if t < 16 else 2
            src = x_d[t * 128:(t + 1) * 128, :] if t < 16 else xh_d
            xtt, xttb = xt.next()
            xnn, xnnb = xn.next()
            ss, ssb = st.next()
            dma("sp", xtt[:rows], src, [], [xttb])
            if NPRE > 4 and t < 12:
                preconv_n(1)
            S.op("act", lambda e: e.activation(
                out=junk[:rows], in_=xtt[:rows], func=AF.Square, accum_out=ss[:rows, 0:1]),
                reads=[xttb], writes=[junkb, ssb])
            rs, rsb = rstd(ss[:rows, 0:1], ssb, 1, 1.0 / D, rows)
            S.op("dve", lambda e: e.scalar_tensor_tensor(
                out=xnn[:rows], in0=xtt[:rows], scalar=rs[:rows, 0:1], in1=gslot[:rows],
                op0=mult, op1=mult), reads=[xttb, rsb, gslotb], writes=[xnnb])
            return (t, rows, xnn, xnnb)

        def a0_s2(ctx):
            t, rows, xnn, xnnb = ctx
            for half in range(2):
                for kk in range(8):
                    k = half * 8 + kk
                    S.op("pe", lambda e, half=half, kk=kk, k=k: e.transpose(
                        out=PT[half][:, kk * 128:kk * 128 + rows],
                        in_=xnn[:rows, k * 128:(k + 1) * 128], identity=ident[:rows, :rows]),
                        reads=[xnnb, identb], writes=[PTb[half]])
                srcp = PT[half][:, :].rearrange("p (k n) -> p k n", k=8)
                if t < 16:
                    dst = xnT[:, half * 8:(half + 1) * 8, t * 128:(t + 1) * 128]
                    wb = xnTb
                else:
                    dst = xnTh[:, half * 8:(half + 1) * 8, :]
                    srcp = srcp[:, :, 0:2]
                    wb = xnThb
                if half == 0:
                    S.op("act", lambda e, dst=dst, srcp=srcp: e.copy(out=dst, in_=srcp),
                         reads=[PTb[half]], writes=[wb])
                else:
                    S.op("dve", lambda e, dst=dst, srcp=srcp: e.tensor_copy(out=dst, in_=srcp),
                         reads=[PTb[half]], writes=[wb])

        def skew(n, s1, s2):
            prev = None
            for i in range(n):
                cur = s1(i)
                if prev is not None:
                    s2(prev)
                prev = cur
            s2(prev)

        skew(17, a0_s1, a0_s2)

        def skew3(n, s1, s2, s3):
            c1 = {}
            c2 = {}
            for i in range(n + 2):
                if i < n:
                    c1[i] = s1(i)
                if 0 <= i - 1 < n:
                    c2[i - 1] = s2(c1.pop(i - 1))
                if 0 <= i - 2 < n:
                    s3(c2.pop(i - 2))

        if phases == 0:
            if debug:
                dma("sp", dbg_d.rearrange("p (k n) -> p k n", k=16), xnT, [xnTb], [S.buf()])
                dma("sp", dbg2_d[:, 0:32].bitcast(BF16).rearrange("p (k n) -> p k n", k=16)[:, :, 0:2], xnTh, [xnThb], [S.buf()])
            finish()
            return nc
        S.barrier()
        A.off = markA
        gu_r = Rot(S, A, 3, 512, F32)
        gv_r = Rot(S, A, 3, 512, F32)
        sq_r = Rot(S, A, 3, 512, F32)
        vn_r = Rot(S, A, 3, 512, BF16)
        ya_r = Rot(S, A, 3, 512, F32)
        yag_r = Rot(S, A, 3, 512, BF16)
        yst_r = Rot(S, A, 3, 512, BF16)

        def a1_s1(i):
            hq, t = divmod(i, TT)
            zfill(2)
            preconv(i)
            if t == 0 and hq not in a1w:
                a1w[hq] = (load_w(hq * 512), load_w(1024 + hq * 512))
            if hq == 1 and t == 2:
                a2w[0] = (load_w(2048), load_w(3072))
            su, sv = a1w[hq]
            pu, pv = (i % 2) * 2, (i % 2) * 2 + 1
            for (pb, sl) in ((pu, su), (pv, sv)):
                for k in range(16):
                    S.op("pe", lambda e, pb=pb, sl=sl, k=k: e.matmul(
                        PM[pb][:, :], lhsT=xnT[:, k, t * 128:(t + 1) * 128], rhs=wslot[sl][:, k, :],
                        start=(k == 0), stop=(k == 15)),
                        reads=[xnTb, wslotb[sl]], writes=[PMb[pb]])
            gu, gub = gu_r.next()
            gv, gvb = gv_r.next()
            sq, sqb = sq_r.next()
            vn, vnb = vn_r.next()
            S.op("act", lambda e: e.activation(out=gv, in_=PM[pv][:, :], func=AF.Gelu_apprx_tanh),
                 reads=[PMb[pv]], writes=[gvb])
            S.op("act", lambda e: e.activation(out=gu, in_=PM[pu][:, :], func=AF.Gelu_apprx_tanh),
                 reads=[PMb[pu]], writes=[gub])
            S.op("dve", lambda e: e.tensor_tensor(out=sq, in0=gv, in1=gv, op=mult),
                 reads=[gvb], writes=[sqb])
            ssv, ssvb = st.next()
            S.op("dve", lambda e: e.tensor_reduce(
                out=ssv[:, 0:4], in_=sq.rearrange("p (h d) -> p h d", h=4), axis=AX.X, op=add),
                reads=[sqb], writes=[ssvb])
            rsv, rsvb = rstd(ssv[:, 0:4], ssvb, 4, 1.0 / 128)
            for h in range(4):
                hs = slice(h * 128, (h + 1) * 128)
                gs = slice(hq * 512 + h * 128, hq * 512 + (h + 1) * 128)
                S.op("dve", lambda e, hs=hs, gs=gs, h=h: e.scalar_tensor_tensor(
                    out=vn[:, hs], in0=gv[:, hs], scalar=rsv[:, h:h + 1], in1=gvn[:, gs],
                    op0=mult, op1=mult), reads=[gvb, rsvb, gvnb], writes=[vnb])
            return (i, hq, t, gu, gub, vn, vnb)

        def a1_s2(ctx):
            i, hq, t, gu, gub, vn, vnb = ctx
            ya, yab = ya_r.next()
            yag, yagb = yag_r.next()
            pmx = 4 + (i % 2)
            for h in range(4):
                hs = slice(h * 128, (h + 1) * 128)
                S.op("pe", lambda e, hs=hs, h=h: e.matmul(
                    PM[pmx][:, hs], lhsT=wsT[:, hq * 4 + h, :], rhs=vn[:, hs], start=True, stop=True),
                    reads=[wsTb, vnb], writes=[PMb[pmx]])
            for h in range(4):
                hs = slice(h * 128, (h + 1) * 128)
                hd = hq * 4 + h
                S.op("dve", lambda e, hs=hs, hd=hd: e.scalar_tensor_tensor(
                    out=ya[:, hs], in0=PM[pmx][:, hs], scalar=bsT[:, hd:hd + 1], in1=gu[:, hs],
                    op0=add, op1=mult), reads=[PMb[pmx], gub, bsTb], writes=[yab])
            c = t * 2 + hq
            S.op("act", lambda e: e.activation(out=junk[:, 0:512], in_=ya, func=AF.Square,
                                               accum_out=ssA[:, c:c + 1]),
                 reads=[yab], writes=[junkb, ssAb])
            S.op("dve", lambda e: e.tensor_tensor(
                out=yag, in0=ya, in1=ga[:, hq * 512:(hq + 1) * 512], op=mult),
                reads=[yab, gab], writes=[yagb])
            return (i, hq, t, yag, yagb)

        def a1_s3(ctx):
            i, hq, t, yag, yagb = ctx
            yst, ystb = yst_r.next()
            pt = i % 2
            for h in range(4):
                hs = slice(h * 128, (h + 1) * 128)
                S.op("pe", lambda e, hs=hs: e.transpose(
                    out=PT[pt][:, hs], in_=yag[:, hs], identity=ident),
                    reads=[yagb, identb], writes=[PTb[pt]])
            S.op("act", lambda e: e.copy(out=yst, in_=PT[pt][:, 0:512]),
                 reads=[PTb[pt]], writes=[ystb])
            dma("sp", yT_d[t, :, hq * 4:(hq + 1) * 4, :], yst.rearrange("p (k n) -> p k n", k=4),
                [ystb], [S.buf()])

        skew3(2 * TT, a1_s1, a1_s2, a1_s3)

        if phases == 1:
            finish()
            return nc
        S.barrier()
        A.off = markA
        cgs_r = Rot(S, A, 2, 512, F32)
        zb_r = Rot(S, A, 2, 514, F32)
        c1_r = Rot(S, A, 2, 512, F32)
        yb_r = Rot(S, A, 2, 512, F32)
        sqb_r = Rot(S, A, 2, 512, F32)
        ybs_r = Rot(S, A, 2, 512, BF16)
        hcs_r = Rot(S, A, 2, 8, F32)
        accB = A.alloc(NT, F32); accBb = S.buf()
        PH = PT[0][:, :].bitcast(F32)
        PHb = PTb[0]
        pset = 0
        for grp in range(2):
            if grp in a2w:
                sb_, sc_ = a2w[grp]
            else:
                sb_ = load_w(2048 + grp * 512)
                sc_ = load_w(3072 + grp * 512)
            sh_ = load_w(4096 + grp * 512)
            for c4 in range(4):
                cb = grp * 4 + c4
                cs = slice(c4 * 128, (c4 + 1) * 128)
                zprev = None
                for tg in range(4):
                    ts_ = slice(tg * 512, (tg + 1) * 512)
                    base = (pset % 2) * 3
                    pset += 1
                    pbg, pcg, phv = base, base + 1, base + 2
                    if tg == 0:
                        for (c0, sl) in ((0, sc_), (2, sh_)):
                            for k in range(16):
                                S.op("pe", lambda e, c0=c0, sl=sl, k=k, cs=cs: e.matmul(
                                    PH[:, c0:c0 + 2], lhsT=wslot[sl][:, k, cs], rhs=xnTh[:, k, :],
                                    start=(k == 0), stop=(k == 15)),
                                    reads=[wslotb[sl], xnThb], writes=[PHb], pe_acc=not (c0 == 0 and k == 0))
                    for (pb, sl) in ((pbg, sb_), (pcg, sc_), (phv, sh_)):
                        for k in range(16):
                            S.op("pe", lambda e, pb=pb, sl=sl, k=k, cs=cs, ts_=ts_: e.matmul(
                                PM[pb][:, :], lhsT=wslot[sl][:, k, cs], rhs=xnT[:, k, ts_],
                                start=(k == 0), stop=(k == 15)),
                                reads=[wslotb[sl], xnTb], writes=[PMb[pb]], pe_acc=(k > 0))
                    zfill(2)
                    preconv(32 + pset - 1)
                    cgs, cgsb = cgs_r.next()
                    zb, zbb = zb_r.next()
                    c1, c1b = c1_r.next()
                    yb, ybb = yb_r.next()
                    sqq, sqqb = sqb_r.next()
                    ybs, ybsb = ybs_r.next()
                    if tg == 0:
                        hcs, hcsb = hcs_r.next()
                        S.op("act", lambda e, hcs=hcs: e.copy(out=hcs[:, 0:2], in_=PH[:, 0:2]),
                             reads=[PHb], writes=[hcsb])
                        S.op("dve", lambda e, zb=zb, hcs=hcs: e.tensor_tensor(
                            out=zb[:, 0:2], in0=hcs[:, 0:2], in1=PH[:, 2:4], op=mult),
                            reads=[hcsb, PHb], writes=[zbb])
                    else:
                        zp, zpb = zprev
                        S.op("dve", lambda e, zb=zb, zp=zp: e.tensor_copy(out=zb[:, 0:2], in_=zp[:, 512:514]),
                             reads=[zpb], writes=[zbb])
                    S.op("act", lambda e, cgs=cgs, pcg=pcg: e.copy(out=cgs, in_=PM[pcg][:, :]),
                         reads=[PMb[pcg]], writes=[cgsb])
                    S.op("dve", lambda e, zb=zb, cgs=cgs, phv=phv: e.tensor_tensor(
                        out=zb[:, 2:514], in0=cgs, in1=PM[phv][:, :], op=mult),
                        reads=[cgsb, PMb[phv]], writes=[zbb])
                    S.op("dve", lambda e, c1=c1, zb=zb, cb=cb: e.tensor_scalar(
                        out=c1, in0=zb[:, 2:514], scalar1=cw[:, cb, 2:3], scalar2=None, op0=mult),
                        reads=[zbb, cwb], writes=[c1b])
                    S.op("dve", lambda e, c1=c1, zb=zb, cb=cb: e.scalar_tensor_tensor(
                        out=c1, in0=zb[:, 1:513], scalar=cw[:, cb, 1:2], in1=c1, op0=mult, op1=add),
                        reads=[zbb, cwb, c1b], writes=[c1b])
                    S.op("dve", lambda e, c1=c1, zb=zb, cb=cb: e.scalar_tensor_tensor(
                        out=c1, in0=zb[:, 0:512], scalar=cw[:, cb, 0:1], in1=c1, op0=mult, op1=add),
                        reads=[zbb, cwb, c1b], writes=[c1b])
                    S.op("dve", lambda e, yb=yb, c1=c1, pbg=pbg: e.tensor_tensor(
                        out=yb, in0=c1, in1=PM[pbg][:, :], op=mult),
                        reads=[c1b, PMb[pbg]], writes=[ybb])
                    S.op("act", lambda e, sqq=sqq, yb=yb: e.activation(out=sqq, in_=yb, func=AF.Square),
                         reads=[ybb], writes=[sqqb])
                    if cb == 0:
                        S.op("dve", lambda e, sqq=sqq, ts_=ts_: e.tensor_copy(out=accB[:, ts_], in_=sqq),
                             reads=[sqqb], writes=[accBb])
                    else:
                        S.op("dve", lambda e, sqq=sqq, ts_=ts_: e.tensor_tensor(
                            out=accB[:, ts_], in0=accB[:, ts_], in1=sqq, op=add),
                            reads=[sqqb, accBb], writes=[accBb])
                    S.op("act", lambda e, ybs=ybs, yb=yb, cb=cb: e.activation(
                        out=ybs, in_=yb, func=AF.Copy, scale=gbt[:, cb:cb + 1]),
                        reads=[ybb, gbtb], writes=[ybsb])
                    dma("sp", yT_d[tg * 4:(tg + 1) * 4, :, 8 + cb, :].rearrange("t p n -> p t n"),
                        ybs.rearrange("p (t n) -> p t n", t=4), [ybsb], [S.buf()])
                    zprev = (zb, zbb)

        ssa2, ssa2b = sm.next()
        S.op("dve", lambda e: e.tensor_reduce(out=ssa2[:, 0:16], in_=ssA.rearrange("p (t q) -> p t q", q=2),
                                              axis=AX.X, op=add), reads=[ssAb], writes=[ssa2b])
        r_, r_b = rstd(ssa2[:, 0:16], ssa2b, 16, 1.0 / 1024)
        S.op("dve", lambda e: e.tensor_copy(out=rsA, in_=r_[:, 0:16]), reads=[r_b], writes=[rsAb])
        for t in range(TT):
            S.op("pe", lambda e, t=t: e.matmul(PM[4][:, t:t + 1], lhsT=accB[:, t * 128:(t + 1) * 128],
                                                rhs=onesf[:, 0:1], start=True, stop=True),
                 reads=[accBb, onesfb], writes=[PMb[4]], pe_acc=(t > 0))
        ssb2, ssb2b = sm.next()
        S.op("dve", lambda e: e.tensor_copy(out=ssb2[:, 0:16], in_=PM[4][:, 0:16]), reads=[PMb[4]], writes=[ssb2b])
        r2_, r2_b = rstd(ssb2[:, 0:16], ssb2b, 16, 1.0 / 1024)
        S.op("dve", lambda e: e.tensor_copy(out=rsB, in_=r2_[:, 0:16]), reads=[r2_b], writes=[rsBb])

        if phases == 2:
            if debug:
                dma("sp", dbg2_d[:, 0:16], rsA, [rsAb], [S.buf()])
                dma("sp", dbg2_d[:, 16:32], rsB, [rsBb], [S.buf()])
            finish()
            return nc
        S.barrier()
        E0_WORDS = 2 * 16 * 512 // 2
        A.off = A.n - E0_WORDS
        wg0 = A.alloc3(16, 512, BF16); wg0b = S.bufs(4)
        wu0 = A.alloc3(16, 512, BF16); wu0b = S.bufs(4)
        A.off = 0
        A_lim = A.n - E0_WORDS
        wout = A.alloc3(16, D, BF16); woutb = S.bufs(4)
        wout_v = wout_d.rearrange("(k p) n -> p k n", p=128)
        for nb in range(4):
            ns = slice(nb * 512, (nb + 1) * 512)
            dma("pool", wout[:, :, ns], wout_v[:, :, ns], [], [woutb[nb]])
        dma("sp", gslot, gffn_d, [], [gslotb])

        def load_gu(dst, src, dstb):
            dflat = dst.rearrange("p k n -> p (k n)")
            sflat = src.rearrange("(p k) n -> p (k n)", k=16)
            for q in range(4):
                dma("pool", dflat[:, q * 2048:(q + 1) * 2048], sflat[:, q * 2048:(q + 1) * 2048], [], [dstb[q]])

        if phases >= 4:
            load_gu(wg0, wg_d[0], wg0b)
            load_gu(wu0, wu_d[0], wu0b)
        yt_r = Rot(S, A, 2, 2048, BF16, a=16)
        xt = Rot(S, A, 2, D, F32)
        ht_r = Rot(S, A, 3, D, F32)
        hn_r = Rot(S, A, 4, D, BF16)
        lg_r = Rot(S, A, 3, 40, F32)
        hnT_r = Rot(S, A, 2, 2048, BF16, a=16)
        tmp_r = Rot(S, A, 2, 512, F32)
        pab = [0]

        assert A.off <= A_lim, ("A3 arena overlaps E0", A.off, A_lim)
        a3ld = {}

        def a3_load(t):
            yt, ytb = yt_r.next()
            xtt, xttb = xt.next()
            dma("sp", yt, yT_d[t], [], [ytb])
            dma("sp", xtt, x_d[t * 128:(t + 1) * 128, :], [], [xttb])
            a3ld[t] = (yt, ytb, xtt, xttb)

        a3_load(0)

        def a3_s1(t):
            if t + 1 < TT:
                a3_load(t + 1)
            yt, ytb, xtt, xttb = a3ld.pop(t)
            ht, htb = ht_r.next()
            for nb in range(4):
                ns = slice(nb * 512, (nb + 1) * 512)
                pa, pb_ = (pab[0] % 2) * 2, (pab[0] % 2) * 2 + 1
                pab[0] += 1
                for (pp, k0) in ((pa, 0), (pb_, 8)):
                    for k in range(k0, k0 + 8):
                        S.op("pe", lambda e, pp=pp, k=k, ns=ns, k0=k0: e.matmul(
                            PM[pp][:, :], lhsT=yt[:, k, :], rhs=wout[:, k, ns],
                            start=(k == k0), stop=(k == k0 + 7)),
                            reads=[ytb, woutb[nb]], writes=[PMb[pp]])
                tmp, tmpb = tmp_r.next()
                hm, hmb = tmp, tmpb
                S.op("act", lambda e, tmp=tmp, pa=pa: e.activation(
                    out=tmp, in_=PM[pa][:, :], func=AF.Copy, scale=rsA[:, t:t + 1]),
                    reads=[PMb[pa], rsAb], writes=[tmpb])
                S.op("dve", lambda e, hm=hm, tmp=tmp, pb_=pb_: e.scalar_tensor_tensor(
                    out=hm, in0=PM[pb_][:, :], scalar=rsB[:, t:t + 1], in1=tmp, op0=mult, op1=add),
                    reads=[PMb[pb_], rsBb, tmpb], writes=[hmb])
                S.op("dve", lambda e, hm=hm, ns=ns: e.tensor_tensor(
                    out=ht[:, ns], in0=hm, in1=xtt[:, ns], op=add),
                    reads=[hmb, xttb], writes=[htb])
            dma("sp", h_d[t * 128:(t + 1) * 128, :], ht, [htb], [S.buf()])
            return (t, ht, htb)

        def a3_sB(ctx):
            t, ht, htb = ctx
            hn, hnb = hn_r.next()
            ss, ssb = st.next()
            S.op("act", lambda e: e.activation(out=junk, in_=ht, func=AF.Square, accum_out=ss[:, 0:1]),
                 reads=[htb], writes=[junkb, ssb])
            rs, rsb = rstd(ss[:, 0:1], ssb, 1, 1.0 / D)
            S.op("dve", lambda e: e.scalar_tensor_tensor(
                out=hn, in0=ht, scalar=rs[:, 0:1], in1=gslot, op0=mult, op1=mult),
                reads=[htb, rsb, gslotb], writes=[hnb])
            return (t, hn, hnb)

        def a3_s2(ctx):
            t, hn, hnb = ctx
            hnT, hnTb = hnT_r.next()
            for half in range(2):
                for kk in range(8):
                    k = half * 8 + kk
                    S.op("pe", lambda e, half=half, kk=kk, k=k: e.transpose(
                        out=PT[half][:, kk * 128:(kk + 1) * 128], in_=hn[:, k * 128:(k + 1) * 128],
                        identity=ident), reads=[hnb, identb], writes=[PTb[half]])
                srcp = PT[half][:, :].rearrange("p (k n) -> p k n", k=8)
                dst = hnT[:, half * 8:(half + 1) * 8, :]
                S.op("act", lambda e, dst=dst, srcp=srcp: e.copy(out=dst, in_=srcp),
                     reads=[PTb[half]], writes=[hnTb])
            for k in range(16):
                S.op("pe", lambda e, hnT=hnT, k=k: e.matmul(
                    PM[4][:, 0:36], lhsT=hnT[:, k, :], rhs=wr[:, k, :], start=(k == 0), stop=(k == 15)),
                    reads=[hnTb, wrb], writes=[PMb[4]], pe_acc=(k > 0))
            lg, lgb = lg_r.next()
            S.op("dve", lambda e, lg=lg: e.tensor_tensor(out=lg[:, 0:36], in0=PM[4][:, 0:36], in1=rb, op=add),
                 reads=[PMb[4], rbb], writes=[lgb])
            return (t, hn, hnb, lg, lgb)

        def a3_s2b(ctx):
            t, hn, hnb, lg, lgb = ctx
            s1, s1b = st.next()
            s2, s2b = st.next()
            wk, wkb = sm.next()
            lem, lemb = sm.next()
            mk1, mk1b = sm.next()
            mk2, mk2b = sm.next()
            V = lambda f, r, w: S.op("dve", f, reads=r, writes=w)
            V(lambda e, lg=lg, s1=s1: e.tensor_reduce(out=s1[:, 0:1], in_=lg[:, 0:4], axis=AX.X, op=ALU.max),
              [lgb], [s1b])
            V(lambda e, lg=lg, s1=s1, t=t: e.tensor_scalar(
                out=rstat[:, t, 0:4], in0=lg[:, 0:4], scalar1=s1[:, 0:1], scalar2=None, op0=ALU.subtract),
              [lgb, s1b], [rstatb[t]])
            V(lambda e, wk=wk, lg=lg, s1=s1: e.tensor_scalar(
                out=wk[:, 4:8], in0=lg[:, 0:4], scalar1=s1[:, 0:1], scalar2=None, op0=ALU.is_ge),
              [lgb, s1b, wkb], [wkb])
            V(lambda e, wk=wk: e.tensor_scalar(out=wk[:, 8:12], in0=wk[:, 4:8], scalar1=-1.0, scalar2=BIG,
                                               op0=add, op1=mult), [wkb], [wkb])
            for g in range(4):
                V(lambda e, lem=lem, lg=lg, wk=wk, g=g: e.tensor_scalar(
                    out=lem[:, g * 8:(g + 1) * 8], in0=lg[:, 4 + g * 8:4 + (g + 1) * 8],
                    scalar1=wk[:, 8 + g:9 + g], scalar2=None, op0=add), [lgb, wkb], [lemb])
            V(lambda e, lem=lem, s1=s1: e.tensor_reduce(out=s1[:, 4:5], in_=lem[:, 0:32], axis=AX.X, op=ALU.max),
              [lemb, s1b], [s1b])
            V(lambda e, mk1=mk1, lem=lem, s1=s1: e.tensor_scalar(
                out=mk1[:, 0:32], in0=lem[:, 0:32], scalar1=s1[:, 4:5], scalar2=None, op0=ALU.is_ge),
              [lemb, s1b], [mk1b])
            V(lambda e, lem=lem, mk1=mk1: e.scalar_tensor_tensor(
                out=lem[:, 0:32], in0=mk1[:, 0:32], scalar=-BIG, in1=lem[:, 0:32], op0=mult, op1=add),
              [mk1b, lemb], [lemb])
            V(lambda e, lem=lem, s1=s1: e.tensor_reduce(out=s1[:, 6:7], in_=lem[:, 0:32], axis=AX.X, op=ALU.max),
              [lemb, s1b], [s1b])
            V(lambda e, mk2=mk2, lem=lem, s1=s1: e.tensor_scalar(
                out=mk2[:, 0:32], in0=lem[:, 0:32], scalar1=s1[:, 6:7], scalar2=None, op0=ALU.is_ge),
              [lemb, s1b], [mk2b])
            V(lambda e, s1=s1, t=t: e.tensor_tensor(out=rstat[:, t, 4:5], in0=s1[:, 6:7], in1=s1[:, 4:5],
                                                    op=ALU.subtract), [s1b], [rstatb[t]])
            V(lambda e, mk1=mk1, mk2=mk2, t=t: e.tensor_tensor(
                out=Aall[:, t, :], in0=mk1[:, 0:32], in1=mk2[:, 0:32], op=add),
              [mk1b, mk2b], [Aallb[t]])
            return (t, hn, hnb, wk, wkb, lem, lemb, mk1, mk1b, mk2, mk2b, s2, s2b)

        def a3_s3(ctx):
            t, hn, hnb, wk, wkb, lem, lemb, mk1, mk1b, mk2, mk2b, s2, s2b = ctx
            V = lambda f, r, w: S.op("dve", f, reads=r, writes=w)
            for tp in range(t + 1):
                S.op("pe", lambda e, tp=tp, t=t: e.matmul(
                    PM[5][:, 0:32], lhsT=(ones if tp < t else utri), rhs=Aall[:, tp, :],
                    start=(tp == 0), stop=(tp == t)),
                    reads=[Aallb[tp], onesb, utrib], writes=[PMb[5]], pe_acc=(tp > 0))
            V(lambda e, wk=wk: e.tensor_tensor(out=wk[:, 0:32], in0=PM[5][:, 0:32], in1=iotc, op=add),
              [PMb[5], iotcb, wkb], [wkb])
            V(lambda e, lem=lem: e.tensor_scalar(out=lem[:, 0:32], in0=PM[5][:, 0:32], scalar1=CAP - 0.5,
                                                 scalar2=1.0e6, op0=ALU.is_ge, op1=mult),
              [PMb[5], lemb], [lemb])
            V(lambda e, wk=wk, lem=lem: e.tensor_tensor(out=wk[:, 0:32], in0=wk[:, 0:32], in1=lem[:, 0:32], op=add),
              [wkb, lemb], [wkb])
            V(lambda e, mk1=mk1, wk=wk: e.tensor_tensor(out=mk1[:, 0:32], in0=mk1[:, 0:32], in1=wk[:, 0:32], op=mult),
              [mk1b, wkb], [mk1b])
            V(lambda e, mk2=mk2, wk=wk: e.tensor_tensor(out=mk2[:, 0:32], in0=mk2[:, 0:32], in1=wk[:, 0:32], op=mult),
              [mk2b, wkb], [mk2b])
            V(lambda e, mk1=mk1, s2=s2: e.tensor_reduce(out=s2[:, 3:4], in_=mk1[:, 0:32], axis=AX.X, op=add),
              [mk1b, s2b], [s2b])
            V(lambda e, mk2=mk2, s2=s2: e.tensor_reduce(out=s2[:, 4:5], in_=mk2[:, 0:32], axis=AX.X, op=add),
              [mk2b, s2b], [s2b])
            V(lambda e, s2=s2, t=t: e.tensor_copy(out=idx[:, t, 0:2], in_=s2[:, 3:5]), [s2b], [idxb[t]])
            for j in range(2):
                S.dma("pool", lambda e, hn=hn, t=t, j=j: e.indirect_dma_start(
                    out=xs_d, out_offset=bass.IndirectOffsetOnAxis(ap=idx[:, t, j:j + 1], axis=0),
                    in_=hn, in_offset=None, bounds_check=bcreg[0], oob_is_err=False),
                    reads=[hnb, idxb[t]], writes=[S.buf()])

        cA, cB, cC = {}, {}, {}
        for c in range(TT + 3):
            if 0 <= c - 3 < TT:
                a3_s3(cC.pop(c - 3))
            if 0 <= c - 2 < TT:
                cC[c - 2] = a3_s2b(a3_s2(cB.pop(c - 2)))
            if 0 <= c - 1 < TT:
                cB[c - 1] = a3_sB(cA.pop(c - 1))
            if c < TT:
                cA[c] = a3_s1(c)

        ge, geb = sm.next()
        g2, g2b = sm.next()
        S.op("act", lambda e: e.activation(out=gex.rearrange("p (t k) -> p t k", k=5), in_=rstat[:, :, 0:5], func=AF.Exp),
             reads=rstatb, writes=[gexb])
        gx3 = gex.rearrange("p (t k) -> p t k", k=5)
        S.op("dve", lambda e: e.tensor_reduce(out=ge[:, 0:16], in_=gx3[:, :, 0:4], axis=AX.X, op=add),
             reads=[gexb], writes=[geb])
        S.op("dve", lambda e: e.reciprocal(out=ge[:, 0:16], in_=ge[:, 0:16]), reads=[geb], writes=[geb])
        S.op("dve", lambda e: e.tensor_scalar(out=g2[:, 0:16], in0=gx3[:, :, 4], scalar1=1.0, scalar2=None, op0=add),
             reads=[gexb], writes=[g2b])
        S.op("dve", lambda e: e.reciprocal(out=g2[:, 0:16], in_=g2[:, 0:16]), reads=[g2b], writes=[g2b])
        S.op("dve", lambda e: e.tensor_tensor(out=g2[:, 16:32], in0=gx3[:, :, 4], in1=g2[:, 0:16], op=mult),
             reads=[gexb, g2b], writes=[g2b])
        S.op("dve", lambda e: e.tensor_tensor(out=gates[:, :, 0], in0=g2[:, 0:16], in1=ge[:, 0:16], op=mult),
             reads=[g2b, geb], writes=gatesb)
        S.op("dve", lambda e: e.tensor_tensor(out=gates[:, :, 1], in0=g2[:, 16:32], in1=ge[:, 0:16], op=mult),
             reads=[g2b, geb], writes=gatesb)

        NA = CAP // 128
        for tp in range(TT):
            S.op("pe", lambda e, tp=tp: e.matmul(PM[5][:, 0:32], lhsT=ones, rhs=Aall[:, tp, :],
                                                 start=(tp == 0), stop=(tp == TT - 1)),
                 reads=[Aallb[tp], onesb], writes=[PMb[5]])
        cnt, cntb = sm.next()
        S.op("dve", lambda e: e.tensor_copy(out=cnt[:, 0:32], in_=PM[5][:, 0:32]), reads=[PMb[5]], writes=[cntb])
        S.op("pool", lambda e: e.iota(rowid, pattern=[[CAP, NE], [128, NA]], base=0, channel_multiplier=1,
                                      allow_small_or_imprecise_dtypes=True), writes=[rowidb])
        S.op("pool", lambda e: e.iota(spos[:, 0:NA], pattern=[[128, NA]], base=0, channel_multiplier=1,
                                      allow_small_or_imprecise_dtypes=True), writes=[sposb])
        for a in range(NA):
            vm, vmb = sm.next()
            vt, vtb = sm.next()
            S.op("dve", lambda e, a=a, vm=vm: e.tensor_scalar(
                out=vm[:, 0:32], in0=cnt[:, 0:32], scalar1=spos[:, a:a + 1], scalar2=None, op0=ALU.is_gt),
                reads=[cntb, sposb], writes=[vmb])
            S.op("dve", lambda e, a=a, vt=vt: e.tensor_scalar(
                out=vt[:, 0:32], in0=rowid[:, :, a], scalar1=-1.0e6, scalar2=None, op0=add),
                reads=[rowidb], writes=[vtb])
            S.op("dve", lambda e, vt=vt, vm=vm: e.tensor_tensor(out=vt[:, 0:32], in0=vt[:, 0:32], in1=vm[:, 0:32], op=mult),
                 reads=[vtb, vmb], writes=[vtb])
            S.op("dve", lambda e, a=a, vt=vt: e.tensor_scalar(
                out=vidx[:, :, a], in0=vt[:, 0:32], scalar1=1.0e6, scalar2=None, op0=add),
                reads=[vtb], writes=[vidxb])
        if debug:
            dma("sp", dbg2_d[:, 32:64], gates.rearrange("p t j -> p (t j)"), gatesb, [S.buf()])
            dma("sp", dbg2_d[:, 64:96].bitcast(I32), idx.rearrange("p t j -> p (t j)"), idxb, [S.buf()])
        if phases == 3:
            finish()
            return nc
        S.barrier()
        A.off = 0
        wgs = [wg0, A.alloc3(16, 512, BF16)]; wgsb = [wg0b, S.bufs(4)]
        wus = [wu0, A.alloc3(16, 512, BF16)]; wusb = [wu0b, S.bufs(4)]
        wds = [A.alloc3(4, D, BF16) for _ in range(2)]; wdsb = [S.bufs(4) for _ in range(2)]
        xg_r = Rot(S, A, 2, NA * D, BF16, a=NA)
        for xg_ in xg_r.t:
            S.op("dve", lambda e, xg_=xg_: e.memset(xg_, 0.0), writes=[xg_r.b[xg_r.t.index(xg_)]])
        xT_r = Rot(S, A, 1, 16 * CAP, BF16, a=16)
        hT_r = Rot(S, A, 2, 4 * CAP, BF16, a=4)
        sg_r = Rot(S, A, 2, CAP, F32)
        yo_r = Rot(S, A, 2, D, BF16)
        assert A.off <= A_lim, ("M arena overlaps E0", A.off, A_lim)

        def load_expert(e_):
            s = e_ % 2
            if 1 <= e_ <= NPRE:
                j = e_ - 1
                for (dst, srcm, dstb) in ((wgs[s], wgb_d[j], wgsb[s]), (wus[s], wub_d[j], wusb[s])):
                    dma("sp", dst.rearrange("p k n -> p (k n)"), srcm.rearrange("(p k) n -> p (k n)", k=16), [], dstb)
                dsrc = wdb_d[j]
            else:
                gsrc, usrc, dsrc = wg_d[e_], wu_d[e_], wd_d[e_]
                if e_ > 0:
                    load_gu(wgs[s], gsrc, wgsb[s])
                    load_gu(wus[s], usrc, wusb[s])
            for k in range(4):
                dma("sp" if 1 <= e_ <= NPRE else "pool", wds[s][:, k, :], dsrc[k * 128:(k + 1) * 128, :], [], [wdsb[s][k]])

        xgs = {}

        def gather_expert(e_):
            xg, xgb = xg_r.next()
            xgs[e_] = (xg, xgb)
            for a in range(NA):
                S.dma("pool", lambda e, xg=xg, a=a, e_=e_: e.indirect_dma_start(
                    out=xg[:, a, :], out_offset=None, in_=xs_d,
                    in_offset=bass.IndirectOffsetOnAxis(ap=vidx[:, e_, a:a + 1], axis=0),
                    bounds_check=bcreg[0], oob_is_err=False), reads=[vidxb], writes=[xgb])

        load_expert(0)
        gather_expert(0)
        gu_i = 0
        py_i = 0
        for e_ in range(NE):
            if e_ + 1 < NE:
                load_expert(e_ + 1)
                gather_expert(e_ + 1)
            s = e_ % 2
            xg, xgb = xgs.pop(e_)
            xT, xTb = xT_r.next()
            hT, hTb = hT_r.next()
            for a in range(NA):
                for half in range(2):
                    for kk in range(8):
                        k = half * 8 + kk
                        S.op("pe", lambda e, xg=xg, a=a, half=half, kk=kk, k=k: e.transpose(
                            out=PT[half][:, kk * 128:(kk + 1) * 128], in_=xg[:, a, k::16],
                            identity=ident), reads=[xgb, identb], writes=[PTb[half]])
                    srcp = PT[half][:, :].rearrange("p (k n) -> p k n", k=8)
                    dst = xT[:, half * 8:(half + 1) * 8, a * 128:(a + 1) * 128]
                    if half == 0:
                        S.op("act", lambda e, dst=dst, srcp=srcp: e.copy(out=dst, in_=srcp),
                             reads=[PTb[half]], writes=[xTb])
                    else:
                        S.op("dve", lambda e, dst=dst, srcp=srcp: e.tensor_copy(out=dst, in_=srcp),
                             reads=[PTb[half]], writes=[xTb])
            for m in range(4):
                ms = slice(m * 128, (m + 1) * 128)
                pg_ = (gu_i % 2) * 2
                pu_ = pg_ + 1
                gu_i += 1
                for (pp, wsl, wslb) in ((pg_, wgs[s], wgsb[s]), (pu_, wus[s], wusb[s])):
                    for k in range(16):
                        S.op("pe", lambda e, pp=pp, wsl=wsl, k=k, ms=ms, xT=xT: e.matmul(
                            PM[pp][:, 0:CAP], lhsT=wsl[:, k, ms], rhs=xT[:, k, :],
                            start=(k == 0), stop=(k == 15)),
                            reads=[wslb[k // 4], xTb], writes=[PMb[pp]])
                sg, sgb = sg_r.next()
                S.op("act", lambda e, sg=sg, pg_=pg_: e.activation(out=sg, in_=PM[pg_][:, 0:CAP], func=AF.Silu),
                     reads=[PMb[pg_]], writes=[sgb])
                S.op("dve", lambda e, hT=hT, m=m, sg=sg, pu_=pu_: e.tensor_tensor(
                    out=hT[:, m, :], in0=sg, in1=PM[pu_][:, 0:CAP], op=mult),
                    reads=[sgb, PMb[pu_]], writes=[hTb])
            for a in range(NA):
                yo, yob = yo_r.next()
                for nb in range(4):
                    ns = slice(nb * 512, (nb + 1) * 512)
                    py = 4 + (py_i % 2)
                    py_i += 1
                    for m in range(4):
                        S.op("pe", lambda e, py=py, hT=hT, m=m, a=a, ns=ns, s=s: e.matmul(
                            PM[py][:, :], lhsT=hT[:, m, a * 128:(a + 1) * 128], rhs=wds[s][:, m, ns],
                            start=(m == 0), stop=(m == 3)),
                            reads=[hTb, wdsb[s][m]], writes=[PMb[py]])
                    if nb % 2 == 0:
                        S.op("act", lambda e, yo=yo, ns=ns, py=py: e.copy(out=yo[:, ns], in_=PM[py][:, :]),
                             reads=[PMb[py]], writes=[yob])
                    else:
                        S.op("dve", lambda e, yo=yo, ns=ns, py=py: e.tensor_copy(out=yo[:, ns], in_=PM[py][:, :]),
                             reads=[PMb[py]], writes=[yob])
                S.dma("pool", lambda e, yo=yo, a=a, e_=e_: e.indirect_dma_start(
                    out=Y_d, out_offset=bass.IndirectOffsetOnAxis(ap=vidx[:, e_, a:a + 1], axis=0),
                    in_=yo, in_offset=None, bounds_check=bcreg[0], oob_is_err=False),
                    reads=[yob, vidxb], writes=[S.buf()])
        if phases == 4:
            finish()
            return nc
        S.barrier()
        A.off = 0
        dma("sp", gslot, gfin_d, [], [gslotb])
        hc_r = Rot(S, A, 4, D, F32)
        y1_r = Rot(S, A, 3, D, BF16)
        y2_r = Rot(S, A, 3, D, BF16)
        ot_r = Rot(S, A, 2, D, F32)
        outbs = []
        cld = {}

        def c_load(t):
            hc, hcb = hc_r.next()
            y1, y1b = y1_r.next()
            y2, y2b = y2_r.next()
            dma("sp", hc, h_d[t * 128:(t + 1) * 128, :], [], [hcb])
            for (j, yy, yyb) in ((0, y1, y1b), (1, y2, y2b)):
                S.dma("pool", lambda e, yy=yy, t=t, j=j: e.indirect_dma_start(
                    out=yy, out_offset=None, in_=Y_d,
                    in_offset=bass.IndirectOffsetOnAxis(ap=idx[:, t, j:j + 1], axis=0),
                    bounds_check=bcreg[0], oob_is_err=False),
                    reads=[idxb[t]], writes=[yyb])
            cld[t] = (hc, hcb, y1, y1b, y2, y2b)

        c_load(0)
        c_load(1)

        def c_x(t):
            hc, hcb, y1, y1b, y2, y2b = cld.pop(t)
            S.op("dve", lambda e: e.scalar_tensor_tensor(
                out=hc, in0=y1, scalar=gates[:, t, 0:1], in1=hc, op0=mult, op1=add),
                reads=[y1b, gatesb[t], hcb], writes=[hcb])
            S.op("dve", lambda e: e.scalar_tensor_tensor(
                out=hc, in0=y2, scalar=gates[:, t, 1:2], in1=hc, op0=mult, op1=add),
                reads=[y2b, gatesb[t], hcb], writes=[hcb])
            ss, ssb = st.next()
            sd, sdb = sm.next()
            S.op("act", lambda e: e.activation(out=junk, in_=hc, func=AF.Square, accum_out=ss[:, 0:1]),
                 reads=[hcb], writes=[junkb, ssb])
            S.op("act", lambda e: e.activation(out=sd[:, 0:1], in_=ss[:, 0:1], func=AF.Sqrt,
                                               scale=1.0 / D, bias=epsb[:, 0:1]),
                 reads=[ssb, epsbb], writes=[sdb])
            return (t, hc, hcb, sd, sdb)

        def c_y(ctx):
            t, hc, hcb, sd, sdb = ctx
            ot, otb = ot_r.next()
            rs, rsb = sm.next()
            S.op("dve", lambda e: e.reciprocal(out=rs[:, 0:1], in_=sd[:, 0:1]), reads=[sdb], writes=[rsb])
            S.op("act", lambda e: e.activation(out=hc, in_=hc, func=AF.Copy, scale=rs[:, 0:1]),
                 reads=[hcb, rsb], writes=[hcb])
            S.op("pool", lambda e: e.tensor_tensor(out=ot, in0=hc, in1=gslot, op=mult),
                 reads=[hcb, gslotb], writes=[otb])
            ob_ = S.buf()
            dma("sp", out_d[t * 128:(t + 1) * 128, :], ot, [otb], [ob_])
            outbs.append(ob_)

        cprev = None
        for t in range(TT):
            if t + 2 < TT:
                c_load(t + 2)
            cur = c_x(t)
            if cprev is not None:
                c_y(cprev)
            cprev = cur
        c_y(cprev)
        S.op("sp", lambda e: None, reads=outbs)
        S.barrier()
        S.emit()
    return nc


_NC_CACHE = {}


def _prep_inputs(inp):
    f = lambda a: np.ascontiguousarray(np.asarray(a, dtype=np.float32))
    bc = lambda v: np.ascontiguousarray(np.broadcast_to(np.asarray(v, np.float32).reshape(1, -1), (128, v.size)))
    x = np.asarray(inp["x"], np.float32)
    shared = {
        "gmix": bc(np.asarray(inp["norm_mix_g"])[0]),
        "gffn": bc(np.asarray(inp["norm_ffn_g"])[0]),
        "gfin": bc(np.asarray(inp["norm_final_g"])),
        "gvn": bc(np.asarray(inp["gmlp_v_norm_g"])[0]),
        "ga": bc(np.asarray(inp["out_norm_gmlp_g"])[0]),
        "gb": f(np.asarray(inp["out_norm_conv_g"])[0].reshape(8, 128).T),
        "wsT": f(np.asarray(inp["gmlp_ws"])[0].transpose(2, 0, 1)),
        "bsT": f(np.asarray(inp["gmlp_bs"])[0].T),
        "cw": f(np.asarray(inp["conv_w"])[0].reshape(3, 8, 128).transpose(2, 1, 0)),
        "w_in": f(np.asarray(inp["w_in"])[0]),
        "w_out": f(np.asarray(inp["w_out"])[0]),
        "wr": f(np.concatenate([np.asarray(inp["router_group_w"])[0], np.asarray(inp["router_expert_w"])[0]],
                               axis=1).reshape(16, 128, 36).transpose(1, 0, 2)),
        "rb": bc(np.concatenate([np.asarray(inp["router_group_b"])[0], np.asarray(inp["router_expert_b"])[0]])),
        "wg": f(np.asarray(inp["expert_w_gate"])[0]),
        "wu": f(np.asarray(inp["expert_w_up"])[0]),
        "wd": f(np.asarray(inp["expert_w_down"])[0]),
    }
    in_maps = []
    for c in range(8):
        b, half = c // 2, c % 2
        s0 = half * NT
        m = dict(shared)
        m["x"] = np.ascontiguousarray(x[b, s0:s0 + NT])
        m["xh"] = np.ascontiguousarray(x[b, s0 - 2:s0]) if half else np.zeros((2, D), np.float32)
        in_maps.append(m)
    return in_maps


def kernel(**inputs):
    in_maps = _prep_inputs(inputs)
    if "nc" not in _NC_CACHE:
        _NC_CACHE["nc"] = build_nc()
    nc = _NC_CACHE["nc"]
    res = run_bass_kernel_spmd(nc, in_maps, core_ids=list(range(8)))
    out = np.empty((4, 4096, D), np.float32)
    for c in range(8):
        b, half = c // 2, c % 2
        out[b, half * NT:(half + 1) * NT] = res.results[c]["out"]
    return out
```
